# Optimizing a Trainium2 kernel written in Bass

```python
import math
import jax, jax.numpy as jnp
from jax import lax
import numpy as np

D_MODEL = 1024
BATCH = 8
SEQ = 4096
DEPTH = 2

GRID_W = 64
CTX_LEN = 256
N_MIXERS = 2
D_INNER = 2 * D_MODEL
HEADDIM = 64
N_SSD_HEADS = D_INNER // HEADDIM
N_GROUPS = 8
HPG = N_SSD_HEADS // N_GROUPS
D_STATE = 128
SSD_CONV = 5
CHUNK = 128
CONV_DIM = D_INNER + 2 * N_GROUPS * D_STATE
D_IN_PROJ = 2 * D_INNER + 2 * N_GROUPS * D_STATE + 2 * N_SSD_HEADS
SC_WIDTH = D_MODEL
SC_K = 3
D_FF = 2816
N_EXPERTS = 8
TOP_K = 2
D_FF_EXPERT = 3584
EPS = 1e-6
N_EVEN = (DEPTH + 1) // 2
N_ODD = DEPTH // 2

kernel_name = "hybrid_ssd_shortconv_moe_dit"


def rmsnorm(x, g):
    xf = x.astype(jnp.float32)
    xf = xf * lax.rsqrt(jnp.mean(xf * xf, axis=-1, keepdims=True) + EPS)
    return (xf * g.astype(jnp.float32)).astype(x.dtype)


def dwconv_centred(u, w):
    k = w.shape[0]
    pad = k // 2
    n = u.shape[-2]
    up = jnp.pad(u, [(0, 0)] * (u.ndim - 2) + [(pad, pad), (0, 0)])
    return sum(up[..., t:t + n, :] * w[t] for t in range(k))


def conv_rows(u, w, rows):
    b, n, ch = u.shape
    return dwconv_centred(u.reshape(b, rows, GRID_W, ch), w).reshape(b, n, ch)


def ssd_chunked(xh, dt, A, Bm, Cm, h0, want_y=True):
    b, n, g, j, p = xh.shape
    ns = Bm.shape[-1]
    nc = n // CHUNK
    xdt = (xh * dt[..., None]).reshape(b, nc, CHUNK, g, j, p)
    Bc = Bm.reshape(b, nc, CHUNK, g, ns)
    Cc = Cm.reshape(b, nc, CHUNK, g, ns)
    dA = (dt * A).reshape(b, nc, CHUNK, g, j).transpose(0, 1, 3, 4, 2)
    acum = jnp.cumsum(dA, axis=-1)
    decay_in = jnp.exp(acum[..., -1:] - acum)
    states = jnp.einsum("bcsgn,bcgjs,bcsgjp->bcgjpn", Bc, decay_in, xdt).astype(jnp.float32)
    chunk_decay = jnp.exp(acum[..., -1])

    def step(h, inp):
        st, dec = inp
        return dec[..., None, None] * h + st, h

    h_final, h_in = lax.scan(step, h0.astype(jnp.float32),
                             (jnp.moveaxis(states, 1, 0), jnp.moveaxis(chunk_decay, 1, 0)))
    if not want_y:
        return None, h_final
    h_in = jnp.moveaxis(h_in, 0, 1)
    mask = jnp.tril(jnp.ones((CHUNK, CHUNK), dtype=bool))
    seg = acum[..., :, None] - acum[..., None, :]
    lmat = jnp.exp(jnp.where(mask, seg, -jnp.inf))
    cb = jnp.einsum("bclgn,bcsgn->bcgls", Cc, Bc)
    y_diag = jnp.einsum("bcgls,bcgjls,bcsgjp->bclgjp", cb, lmat, xdt)
    y_off = jnp.einsum("bclgn,bcgjpn,bcgjl->bclgjp", Cc, h_in, jnp.exp(acum))
    return (y_diag + y_off).reshape(b, n, g, j, p), h_final


def ssd_mixer(h_lat, h_ctx, rows, w_in, conv_w, conv_b, a_log, dt_bias, d_skip, norm_w, w_out, ctx_out):
    A = -jnp.exp(a_log.astype(jnp.float32)).reshape(2, N_GROUPS, HPG)

    def prep(h, conv_fn):
        b, n = h.shape[:2]
        z, xbc, dt_raw = jnp.split(h @ w_in, [D_INNER, D_INNER + CONV_DIM], axis=-1)
        xbc = jax.nn.silu(conv_fn(xbc) + conv_b)
        xs, bm, cm = jnp.split(xbc, [D_INNER, D_INNER + N_GROUPS * D_STATE], axis=-1)
        xs = xs.reshape(b, n, N_GROUPS, HPG, HEADDIM)
        bm = bm.reshape(b, n, N_GROUPS, D_STATE)
        cm = cm.reshape(b, n, N_GROUPS, D_STATE)
        dt = jax.nn.softplus((dt_raw + dt_bias.reshape(-1)).astype(jnp.float32))
        return z, xs, bm, cm, dt.reshape(b, n, 2, N_GROUPS, HPG)

    def finish(z, xs, y):
        b, n = z.shape[:2]
        y = y + d_skip.reshape(N_GROUPS, HPG, 1) * xs
        y = y.reshape(b, n, D_INNER).astype(z.dtype) * jax.nn.silu(z)
        y = rmsnorm(y.reshape(b, n, N_GROUPS, D_INNER // N_GROUPS),
                    norm_w.reshape(N_GROUPS, D_INNER // N_GROUPS)).reshape(b, n, D_INNER)
        return y @ w_out

    zc, xc, bc, cc, dtc = prep(h_ctx, lambda u: dwconv_centred(u, conv_w))
    zl, xl, bl, cl, dtl = prep(h_lat, lambda u: conv_rows(u, conv_w, rows))
    b = h_lat.shape[0]
    y_lat = jnp.zeros(xl.shape, jnp.float32)
    y_ctx = jnp.zeros(xc.shape, jnp.float32) if ctx_out else None
    for d in range(2):
        o = (lambda t: t) if d == 0 else (lambda t: jnp.flip(t, axis=1))
        h0 = jnp.zeros((b, N_GROUPS, HPG, HEADDIM, D_STATE), jnp.float32)
        yc, hc = ssd_chunked(o(xc), o(dtc[:, :, d]), A[d], o(bc), o(cc), h0, want_y=ctx_out)
        yl, _ = ssd_chunked(o(xl), o(dtl[:, :, d]), A[d], o(bl), o(cl), hc)
        y_lat = y_lat + o(yl)
        if ctx_out:
            y_ctx = y_ctx + o(yc)
    out_lat = finish(zl, xl, y_lat)
    out_ctx = finish(zc, xc, y_ctx) if ctx_out else None
    return out_lat, out_ctx


def shortconv_mixer(h_lat, h_ctx, rows, w_in, conv_w, w_out):
    def one(h, conv_fn):
        gb, gc, v = jnp.split(h @ w_in, 3, axis=-1)
        return (gb * conv_fn(gc * v)) @ w_out

    out_lat = one(h_lat, lambda u: conv_rows(u, conv_w, rows))
    out_ctx = one(h_ctx, lambda u: dwconv_centred(u, conv_w)) if h_ctx is not None else None
    return out_lat, out_ctx


def swiglu(h, w_gu, w_down):
    g, u = jnp.split(h @ w_gu, 2, axis=-1)
    return (jax.nn.silu(g) * u) @ w_down


def moe_ffn(h, router, w_gu, w_down):
    logits = (h @ router).astype(jnp.float32)
    top_val, top_idx = lax.top_k(logits, TOP_K)
    gates = jax.nn.softmax(top_val, axis=-1)
    out = jnp.zeros_like(h)
    for e in range(N_EXPERTS):
        w_e = jnp.sum(jnp.where(top_idx == e, gates, 0.0), axis=-1, keepdims=True).astype(h.dtype)
        out = out + w_e * swiglu(h, w_gu[e], w_down[e])
    return out


def setup_inputs(seed: int = 0) -> dict:
    key = jax.random.key(seed)
    ks = jax.random.split(key, 24)
    f32 = jnp.float32

    def nrm(k, shape, scale):
        return jax.random.normal(k, shape, f32) * scale

    dt0 = jnp.exp(jax.random.uniform(ks[12], (N_EVEN, 2, N_SSD_HEADS), f32, math.log(1e-3), math.log(1e-1)))
    return {
        "x": nrm(ks[0], (BATCH, SEQ, D_MODEL), 1.0),
        "c": nrm(ks[1], (BATCH, D_MODEL), 1.0),
        "ctx": nrm(ks[2], (BATCH, CTX_LEN, D_MODEL), 1.0),
        "c_ctx": nrm(ks[3], (D_MODEL,), 1.0),
        "ada_w": nrm(ks[4], (DEPTH, D_MODEL, 6 * D_MODEL), 0.5 * D_MODEL ** -0.5),
        "ada_b": nrm(ks[5], (DEPTH, 6 * D_MODEL), 0.02),
        "norm_pre": 1.0 + nrm(ks[6], (DEPTH, 2, D_MODEL), 0.1),
        "norm_post": 1.0 + nrm(ks[7], (DEPTH, 2, D_MODEL), 0.1),
        "ssd_w_in": nrm(ks[8], (N_EVEN, D_MODEL, D_IN_PROJ), D_MODEL ** -0.5),
        "ssd_conv_w": nrm(ks[9], (N_EVEN, SSD_CONV, CONV_DIM), SSD_CONV ** -0.5),
        "ssd_conv_b": nrm(ks[10], (N_EVEN, CONV_DIM), 0.02),
        "ssd_a_log": jnp.log(jax.random.uniform(ks[11], (N_EVEN, 2, N_SSD_HEADS), f32, 1.0, 16.0)),
        "ssd_dt_bias": dt0 + jnp.log(-jnp.expm1(-dt0)),
        "ssd_d": 1.0 + nrm(ks[13], (N_EVEN, N_SSD_HEADS), 0.1),
        "ssd_norm": 1.0 + nrm(ks[14], (N_EVEN, D_INNER), 0.1),
        "ssd_w_out": nrm(ks[15], (N_EVEN, D_INNER, D_MODEL), D_INNER ** -0.5),
        "sc_w_in": nrm(ks[16], (N_ODD, D_MODEL, 3 * SC_WIDTH), D_MODEL ** -0.5),
        "sc_conv_w": nrm(ks[17], (N_ODD, SC_K, SC_WIDTH), SC_K ** -0.5),
        "sc_w_out": nrm(ks[18], (N_ODD, SC_WIDTH, D_MODEL), SC_WIDTH ** -0.5),
        "ffn_w_gu": nrm(ks[19], (N_EVEN, D_MODEL, 2 * D_FF), D_MODEL ** -0.5),
        "ffn_w_down": nrm(ks[20], (N_EVEN, D_FF, D_MODEL), D_FF ** -0.5),
        "moe_router": nrm(ks[21], (N_ODD, D_MODEL, N_EXPERTS), D_MODEL ** -0.5),
        "moe_w_gu": nrm(ks[22], (N_ODD, N_EXPERTS, D_MODEL, 2 * D_FF_EXPERT), D_MODEL ** -0.5),
        "moe_w_down": nrm(ks[23], (N_ODD, N_EXPERTS, D_FF_EXPERT, D_MODEL), D_FF_EXPERT ** -0.5),
    }


def reference(x, c, ctx, c_ctx, ada_w, ada_b, norm_pre, norm_post, ssd_w_in, ssd_conv_w, ssd_conv_b,
              ssd_a_log, ssd_dt_bias, ssd_d, ssd_norm, ssd_w_out, sc_w_in, sc_conv_w, sc_w_out,
              ffn_w_gu, ffn_w_down, moe_router, moe_w_gu, moe_w_down):
    rows = x.shape[1] // GRID_W
    x_lat, x_ctx = x, ctx
    s_c = jax.nn.silu(c)
    s_cc = jax.nn.silu(c_ctx)
    for i in range(DEPTH):
        j = i // N_MIXERS
        is_ssd = i % N_MIXERS == 0
        ctx_next = any(k % N_MIXERS == 0 for k in range(i + 1, DEPTH))
        need_ctx_in = is_ssd or ctx_next
        sh1, sc1, g1, sh2, sc2, g2 = jnp.split((s_c @ ada_w[i] + ada_b[i])[:, None, :], 6, axis=-1)
        csh1, csc1, cg1, csh2, csc2, cg2 = jnp.split((s_cc @ ada_w[i] + ada_b[i])[None, None, :], 6, axis=-1)

        h_lat = rmsnorm(x_lat, norm_pre[i, 0]) * (1.0 + sc1) + sh1
        h_ctx = rmsnorm(x_ctx, norm_pre[i, 0]) * (1.0 + csc1) + csh1 if need_ctx_in else None
        if is_ssd:
            y_lat, y_ctx = ssd_mixer(h_lat, h_ctx, rows, ssd_w_in[j], ssd_conv_w[j], ssd_conv_b[j], ssd_a_log[j],
                                     ssd_dt_bias[j], ssd_d[j], ssd_norm[j], ssd_w_out[j], ctx_next)
        else:
            y_lat, y_ctx = shortconv_mixer(h_lat, h_ctx, rows, sc_w_in[j], sc_conv_w[j], sc_w_out[j])
        x_lat = x_lat + g1 * rmsnorm(y_lat, norm_post[i, 0])
        if ctx_next:
            x_ctx = x_ctx + cg1 * rmsnorm(y_ctx, norm_post[i, 0])

        if is_ssd:
            ffn = lambda h: swiglu(h, ffn_w_gu[j], ffn_w_down[j])
        else:
            ffn = lambda h: moe_ffn(h, moe_router[j], moe_w_gu[j], moe_w_down[j])
        x_lat = x_lat + g2 * rmsnorm(ffn(rmsnorm(x_lat, norm_pre[i, 1]) * (1.0 + sc2) + sh2), norm_post[i, 1])
        if ctx_next:
            x_ctx = x_ctx + cg2 * rmsnorm(ffn(rmsnorm(x_ctx, norm_pre[i, 1]) * (1.0 + csc2) + csh2), norm_post[i, 1])
    return x_lat
```

```python
import numpy as np
import concourse.bass as bass
import concourse.mybir as mybir
from concourse.bass_utils import run_bass_kernel_spmd
from contextlib import ExitStack

F32 = mybir.dt.float32
BF16 = mybir.dt.bfloat16
AF = mybir.ActivationFunctionType
ALU = mybir.AluOpType

ENGS = ("pe", "act", "dve", "pool", "sp")
SAME_ENGINE_SYNC = {"pe": False, "act": True, "dve": True, "pool": True, "sp": False}

D = 1024
KD = 8
TC = 256
DI = 2048
NH = 32
NG = 8
HP = 64
DIN = 6208
DFF = 2816
NE = 8
DFE = 3584
EPS = 1e-6
NEG = -30000.0
NSIDE = 2


class Prog:
    def __init__(self, nc, n_dma_sems=48):
        self.nc = nc
        self.ops = []
        self.eng_sem = {e: nc.alloc_semaphore("prog_" + e) for e in ("pe", "act", "dve", "pool")}
        self.eng_cnt = {e: 0 for e in ("pe", "act", "dve", "pool")}
        self.dma_sems = [nc.alloc_semaphore("dma%d" % i) for i in range(n_dma_sems)]
        self.dma_val = [0] * n_dma_sems
        self.dma_rr = 0
        self.dma_rr_sw = 0
        self.last_w = {}
        self.readers = {}
        self.waited = {e: {} for e in ENGS}
        self.region = None
        self.regs = {}

    def begin_cond(self, cond_ap, dep_reads):
        self.region = dict(cond=cond_ap, deps=list(dep_reads), pre={}, start_cnt=dict(self.eng_cnt))
        self._snap = {e: dict(w) for e, w in self.waited.items()}

    def end_cond(self):
        self.region = None
        self.waited = self._snap

    def add(self, eng, fn, reads=(), writes=(), dma=False, n_dma=1):
        op = dict(eng=eng, fn=fn, dma=dma, waits=[], n_dma=n_dma, region=self.region)
        if self.region is not None and eng not in self.region["pre"]:
            pre = dict(eng=eng, waits=[])
            for r in self.region["deps"]:
                w = self.last_w.get(r)
                if w is not None:
                    self._want(pre, *w["sig"])
            self.region["pre"][eng] = pre["waits"]
        deps = []
        for r in reads:
            w = self.last_w.get(r)
            if w is not None:
                deps.append(w)
        for r in writes:
            w = self.last_w.get(r)
            if w is not None:
                deps.append(w)
            deps.extend(self.readers.get(r, ()))
        for r in reads:
            self.readers.setdefault(r, []).append(op)
        for r in writes:
            self.last_w[r] = op
            self.readers[r] = []
        if dma:
            n_sw = 16
            if eng == "pool":
                si = self.dma_rr_sw
                self.dma_rr_sw = (self.dma_rr_sw + 1) % n_sw
            else:
                si = n_sw + self.dma_rr
                self.dma_rr = (self.dma_rr + 1) % (len(self.dma_sems) - n_sw)
            prev = self.dma_val[si]
            op["dma_prev"] = (si, prev)
            if prev > 0:
                self._want(op, ("dma", si), prev)
            self.dma_val[si] = prev + 16 * n_dma
            op["sig"] = (("dma", si), self.dma_val[si])
        else:
            op["cnt_before"] = self.eng_cnt[eng]
            self.eng_cnt[eng] += 1
            op["sig"] = (("eng", eng), self.eng_cnt[eng])
        for d in deps:
            if d is op:
                continue
            key, val = d["sig"]
            if key[0] == "eng" and key[1] == eng and not SAME_ENGINE_SYNC[eng]:
                continue
            self._want(op, key, val)
        self.ops.append(op)
        return op

    def _want(self, op, key, val):
        w = self.waited[op["eng"]]
        if w.get(key, 0) >= val:
            return
        w[key] = val
        op["waits"].append((key, val))

    def _sem(self, key):
        return self.eng_sem[key[1]] if key[0] == "eng" else self.dma_sems[key[1]]

    def flush(self):
        nc = self.nc
        ops = self.ops
        self.ops = []
        by_eng = {e: [o for o in ops if o["eng"] == e] for e in ENGS}
        finals = [(("eng", e), self.eng_cnt[e]) for e in self.eng_cnt if self.eng_cnt[e] > 0]
        finals += [(("dma", i), v) for i, v in enumerate(self.dma_val) if v > 0]

        def emit_op(eng, o):
            for key, val in o["waits"]:
                eng.wait_ge(self._sem(key), val)
            r = o["fn"](eng)
            key, val = o["sig"]
            if o["dma"]:
                rs = r if isinstance(r, (list, tuple)) else [r]
                assert len(rs) == o["n_dma"], (len(rs), o["n_dma"])
                for i in rs:
                    i.then_inc(self._sem(key), 16)
            else:
                assert r is not None
                r.then_inc(self._sem(key), 1)

        def emit(eng_name, eng):
            lst = by_eng[eng_name]
            i = 0
            while i < len(lst):
                o = lst[i]
                R = o["region"]
                if R is None:
                    emit_op(eng, o)
                    i += 1
                    continue
                j = i
                while j < len(lst) and lst[j]["region"] is R:
                    j += 1
                group = lst[i:j]
                i = j
                for key, val in R["pre"][eng_name]:
                    eng.wait_ge(self._sem(key), val)
                if eng_name not in self.regs:
                    self.regs[eng_name] = eng.alloc_register("cond_" + eng_name)
                reg = self.regs[eng_name]
                eng.reg_load(reg, R["cond"])
                with eng.If_ne(reg, 0):
                    for o2 in group:
                        emit_op(eng, o2)
                with eng.Else():
                    ncomp = [o2 for o2 in group if not o2["dma"]]
                    if ncomp:
                        c0 = ncomp[0]["cnt_before"]
                        if c0 > 0:
                            eng.wait_ge(self.eng_sem[eng_name], c0)
                        eng.sem_inc(self.eng_sem[eng_name], len(ncomp))
                    for o2 in group:
                        if o2["dma"]:
                            si, prev = o2["dma_prev"]
                            if prev > 0:
                                eng.wait_ge(self.dma_sems[si], prev)
                            eng.sem_inc(self.dma_sems[si], 16 * o2["n_dma"])
            w = self.waited[eng_name]
            for key, val in finals:
                if key == ("eng", eng_name):
                    continue
                if w.get(key, 0) < val:
                    w[key] = val
                    eng.wait_ge(self._sem(key), val)

        with nc.Block() as block:
            @block.tensor
            def _(e):
                emit("pe", e)

            @block.scalar
            def _(e):
                emit("act", e)

            @block.vector
            def _(e):
                emit("dve", e)

            @block.gpsimd
            def _(e):
                emit("pool", e)

            @block.sync
            def _(e):
                emit("sp", e)
        self.last_w = {}
        self.readers = {}


def bview(ap, h, q):
    return ap.rearrange("p (h q) -> p h q", h=h)


def build(T=4096, phases="ABCDE", dbg=False, e_stop=3, d_stop=99):
    NCH = T // 128
    TT = T + TC
    nc = bass.Bass("TRN2", target_bir_lowering=False)
    dt_ = nc.dram_tensor

    def inp(name, shape):
        return dt_(name, list(shape), F32, kind="ExternalInput").ap()

    x_in = inp("x", [T, D])
    c_in = inp("c", [D])
    ctx_in = inp("ctx", [TC, D])
    cctx_in = inp("c_ctx", [D])
    ada_w = inp("ada_w", [2, D, 6 * D])
    ada_b = inp("ada_b", [2, 6 * D])
    norm_pre = inp("norm_pre", [2, 2 * D])
    norm_post = inp("norm_post", [2, 2 * D])
    ssd_w_in = inp("ssd_w_in", [D, DIN])
    ssd_conv_w = inp("ssd_conv_w", [5, 4096])
    ssd_conv_b = inp("ssd_conv_b", [4096])
    ssd_a_log = inp("ssd_a_log", [1, 64])
    ssd_dt_bias = inp("ssd_dt_bias", [1, 64])
    ssd_d = inp("ssd_d", [1, 32])
    ssd_norm = inp("ssd_norm", [1, DI])
    ssd_w_out = inp("ssd_w_out", [DI, D])
    sc_w_in = inp("sc_w_in", [D, 3 * D])
    sc_conv_w = inp("sc_conv_w", [3, D])
    sc_w_out = inp("sc_w_out", [D, D])
    ffn_w_gu = inp("ffn_w_gu", [D, 2 * DFF])
    ffn_w_down = inp("ffn_w_down", [DFF, D])
    moe_router = inp("moe_router", [D, NE])
    moe_w_gu = inp("moe_w_gu", [NE, D, 2 * DFE])
    moe_w_down = inp("moe_w_down", [NE, DFE, D])
    out = dt_("out", [T, D], F32, kind="ExternalOutput").ap()

    sk = "ExternalOutput" if dbg else "Internal"
    modv = dt_("modv", [2, 2, 3, 2, D], F32, kind=sk).ap()
    x_tok = dt_("x_tok", [TT, DI], BF16, kind=sk).ap()
    B_tok = dt_("B_tok", [TT, 1024], BF16, kind=sk).ap()
    BT = dt_("BT", [1024, T], BF16, kind=sk).ap()
    CT = dt_("CT", [1024, T], BF16, kind=sk).ap()
    sz = dt_("sz", [T, DI], BF16, kind=sk).ap()
    dts = dt_("dts", [TT, 64], F32, kind=sk).ap()
    hb = dt_("hb", [NCH, 128, DI], BF16, kind=sk).ap()
    x1 = dt_("x1", [T, D], F32, kind=sk).ap()
    hf0 = dt_("hf0", [128, DI], F32, kind=sk).ap()
    if dbg:
        dbg_ffn = dt_("dbg_ffn", [T, D], F32, kind=sk).ap()
        dbg_sc = dt_("dbg_sc", [T, D], F32, kind=sk).ap()
        dbg_wg = dt_("dbg_wg", [T, NE], F32, kind=sk).ap()

    P = Prog(nc)
    ps = nc.alloc_psum_tensor("ps", [128, 8, 512], F32)
    identb = nc.alloc_sbuf_tensor("identb", [128, 128], BF16)
    identf = nc.alloc_sbuf_tensor("identf", [128, 128], F32)

    def psb(b):
        return ps[:, b, :]

    def psb16(b):
        return ps[:, b, :].bitcast(BF16)

    def dma(eng, out_ap, in_ap, reads, writes):
        P.add(eng, lambda e: e.dma_start(out=out_ap, in_=in_ap), reads=reads, writes=writes, dma=True)

    def dma_slow(eng, out_ap, in_ap, reads, writes):
        P.add(eng, lambda e: e.dma_start(out=out_ap, in_=in_ap, allow_slow_non_contiguous=True),
              reads=reads, writes=writes, dma=True)

    def seq(eng, fns, reads, writes):
        for f in fns:
            P.add(eng, f, reads=reads, writes=writes)

    seq("pool", [
        lambda e: e.memset(identf[:], 0.0),
        lambda e: e.affine_select(out=identf[:], in_=identf[:], pattern=[[-1, 128]], compare_op=ALU.not_equal,
                                  fill=1.0, base=0, channel_multiplier=1),
        lambda e: e.memset(identb[:], 0.0),
        lambda e: e.affine_select(out=identb[:], in_=identb[:], pattern=[[-1, 128]], compare_op=ALU.not_equal,
                                  fill=1.0, base=0, channel_multiplier=1),
    ], [], ["ident"])
    P.flush()

    def rstd_ops(src_ap, junk_ap, ss_ap, rs_ap, n, rd, tag):
        P.add("act", lambda e: e.activation(out=junk_ap, in_=src_ap, func=AF.Square, accum_out=ss_ap),
              reads=rd, writes=[tag + "junk", tag + "ss"])
        P.add("act", lambda e: e.activation(out=rs_ap, in_=ss_ap, func=AF.Sqrt, bias=EPS, scale=1.0 / n),
              reads=[tag + "ss"], writes=[tag + "rs"])
        P.add("dve", lambda e: e.reciprocal(out=rs_ap, in_=rs_ap), reads=[tag + "rs"], writes=[tag + "rs"])

    def norm_mod(x_ap, x_res, A_t, B_t, hn_ap, tmp, junk, st, tag, hn32=None, par=0):
        tag = tag + ("p%d" % par if par else "")
        c0 = 8 * par
        rstd_ops(x_ap, junk[:], st[:, c0:c0 + 1], st[:, c0 + 1:c0 + 2], D, x_res, tag)
        P.add("dve", lambda e: e.scalar_tensor_tensor(out=tmp[:], in0=x_ap, scalar=st[:, c0 + 1:c0 + 2], in1=A_t[:],
                                                      op0=ALU.mult, op1=ALU.mult),
              reads=x_res + [tag + "rs", "modtiles"], writes=[tag + "tmp"])
        if hn32 is not None:
            P.add("pool", lambda e: e.tensor_tensor(out=hn32, in0=tmp[:], in1=B_t[:], op=ALU.add),
                  reads=[tag + "tmp", "modtiles"], writes=[tag + "hn32"])
            P.add("act", lambda e: e.activation(out=hn_ap, in_=hn32, func=AF.Copy),
                  reads=[tag + "hn32"], writes=[tag + "hn"])
        else:
            P.add("pool", lambda e: e.tensor_tensor(out=hn_ap, in0=tmp[:], in1=B_t[:], op=ALU.add),
                  reads=[tag + "tmp", "modtiles"], writes=[tag + "hn"])
        return tag + "hn"

    def transposes_to(hn_ap, nk, bank, dst_ap, rd, wr, eng="act"):
        nb = (nk + 7) // 8
        banks = [bank + i for i in range(nb)]

        def tr(e):
            for k in range(nk):
                r = e.transpose(out=psb16(bank + k // 8)[:, (k % 8) * 128:(k % 8 + 1) * 128],
                                in_=hn_ap[:, k * 128:(k + 1) * 128], identity=identb[:])
            return r
        P.add("pe", tr, reads=rd + ["ident"], writes=["ps%d" % b for b in banks])
        for i, b in enumerate(banks):
            k0 = i * 8
            k1 = min(nk, k0 + 8)
            src = psb16(b)[:, 0:(k1 - k0) * 128].rearrange("p (k t) -> p k t", t=128)
            dsl = dst_ap[:, k0:k1, :]
            if eng == "act":
                P.add("act", lambda e, s=src, d_=dsl: e.activation(out=d_, in_=s, func=AF.Copy),
                      reads=["ps%d" % b], writes=wr)
            else:
                P.add("dve", lambda e, s=src, d_=dsl: e.tensor_copy(out=d_, in_=s),
                      reads=["ps%d" % b], writes=wr)

    def load_mod(i, s, r, A_t, B_t, G_t):
        for q, t in ((0, A_t), (1, B_t), (2, G_t)):
            if t is None:
                continue
            dma("sp", t[:], modv[i, s, q, r:r + 1, :].partition_broadcast(128), ["modv"], ["modtiles"])

    def post_norm_res(y_ap, y_res, G_t, xres_ap, xres_res, tmp, junk, st, tag, par=0):
        tag = tag + ("q%d" % par if par else "")
        c0 = 8 * par
        rstd_ops(y_ap, junk[:], st[:, c0 + 2:c0 + 3], st[:, c0 + 3:c0 + 4], D, y_res, tag + "p")
        P.add("dve", lambda e: e.scalar_tensor_tensor(out=tmp[:], in0=y_ap, scalar=st[:, c0 + 3:c0 + 4], in1=G_t[:],
                                                      op0=ALU.mult, op1=ALU.mult),
              reads=y_res + [tag + "prs", "modtiles"], writes=[tag + "tmp"])
        P.add("pool", lambda e: e.tensor_tensor(out=xres_ap, in0=xres_ap, in1=tmp[:], op=ALU.add),
              reads=[tag + "tmp"] + xres_res, writes=xres_res)

    def transposes_list(in_aps, bank, dst_ap, rd, wr, eng="act"):
        nk = len(in_aps)
        nb = (nk + 7) // 8
        banks = [bank + i for i in range(nb)]

        def tr(e):
            for k in range(nk):
                r = e.transpose(out=psb16(bank + k // 8)[:, (k % 8) * 128:(k % 8 + 1) * 128],
                                in_=in_aps[k], identity=identb[:])
            return r
        P.add("pe", tr, reads=rd + ["ident"], writes=["ps%d" % b for b in banks])
        for i, b in enumerate(banks):
            k0 = i * 8
            k1 = min(nk, k0 + 8)
            src = psb16(b)[:, 0:(k1 - k0) * 128].rearrange("p (k t) -> p k t", t=128)
            dsl = dst_ap[:, k0:k1, :]
            if eng == "act":
                P.add("act", lambda e, s=src, d_=dsl: e.activation(out=d_, in_=s, func=AF.Copy),
                      reads=["ps%d" % b], writes=wr)
            else:
                P.add("dve", lambda e, s=src, d_=dsl: e.tensor_copy(out=d_, in_=s),
                      reads=["ps%d" % b], writes=wr)

    tri_id = [0]

    def tri_consts(es):
        tri_id[0] += 1
        Lex = es.enter_context(nc.sbuf_tensor("Lex%d" % tri_id[0], [128, 128], F32))
        Uex = es.enter_context(nc.sbuf_tensor("Uex%d" % tri_id[0], [128, 128], F32))
        ones = es.enter_context(nc.sbuf_tensor("ones%d" % tri_id[0], [128, 128], F32))
        A_b = es.enter_context(nc.sbuf_tensor("A_b%d" % tri_id[0], [128, 64], F32))

        seq("pool", [
            lambda e: e.memset(ones[:], 1.0),
            lambda e: e.memset(Lex[:], 1.0),
            lambda e: e.affine_select(out=Lex[:], in_=Lex[:], pattern=[[1, 128]], compare_op=ALU.is_gt, fill=0.0,
                                      base=0, channel_multiplier=-1),
            lambda e: e.memset(Uex[:], 1.0),
            lambda e: e.affine_select(out=Uex[:], in_=Uex[:], pattern=[[-1, 128]], compare_op=ALU.is_gt, fill=0.0,
                                      base=0, channel_multiplier=1),
        ], [], ["tri"])
        dma("sp", A_b[:], ssd_a_log[0:1, :].partition_broadcast(128), [], ["A_b"])
        P.add("act", lambda e: e.activation(out=A_b[:], in_=A_b[:], func=AF.Exp), reads=["A_b"], writes=["A_b"])
        P.add("dve", lambda e: e.tensor_scalar(out=A_b[:], in0=A_b[:], scalar1=-1.0, scalar2=None, op0=ALU.mult),
              reads=["A_b"], writes=["A_b"])
        return Lex, Uex, ones, A_b

    def phase_A():
        with ExitStack() as es:
            def sb(name, shape, dt):
                return es.enter_context(nc.sbuf_tensor(name, shape, dt))
            cT = sb("cT", [128, 8, 2], F32)
            wa = sb("wa", [128, 2, 8, 512], F32)
            modraw = sb("modraw", [2, 6 * D], F32)
            adab = sb("adab", [2, 6 * D], F32)
            npre = sb("npre", [2, 2 * D], F32)
            npost = sb("npost", [2, 2 * D], F32)
            outv = sb("outv", [2, 6, D], F32)
            dma_slow("sp", cT[:, :, 0], c_in.rearrange("(k p) -> p k", p=128), [], ["cT"])
            dma_slow("sp", cT[:, :, 1], cctx_in.rearrange("(k p) -> p k", p=128), [], ["cT"])
            P.add("act", lambda e: e.activation(out=cT[:], in_=cT[:], func=AF.Silu), reads=["cT"], writes=["cT"])
            for i in range(2):
                dma("sp", adab[:], ada_b[i:i + 1, :].partition_broadcast(2), [], ["adab"])
                dma("sp", npre[:], norm_pre[i:i + 1, :].partition_broadcast(2), [], ["npre"])
                dma("sp", npost[:], norm_post[i:i + 1, :].partition_broadcast(2), [], ["npost"])
                wv = ada_w[i].rearrange("(k p) n -> p k n", p=128)
                for ct in range(12):
                    b = ct % 2
                    dma("sp", wa[:, b], wv[:, :, ct * 512:(ct + 1) * 512], [], ["wa%d" % b])

                    def mm(e, b=b):
                        for k in range(8):
                            r = e.matmul(ps[0:2, b, :], lhsT=cT[:, k, :], rhs=wa[:, b, k, :], start=(k == 0), stop=(k == 7))
                        return r
                    P.add("pe", mm, reads=["cT", "wa%d" % b], writes=["ps%d" % b])
                    P.add("dve", lambda e, b=b, ct=ct: e.tensor_tensor(out=modraw[:, ct * 512:(ct + 1) * 512], in0=ps[0:2, b, :],
                                                                       in1=adab[:, ct * 512:(ct + 1) * 512], op=ALU.add),
                          reads=["ps%d" % b, "adab"], writes=["modraw"])
                for s in range(2):
                    sh = modraw[:, (3 * s) * D:(3 * s + 1) * D]
                    sc = modraw[:, (3 * s + 1) * D:(3 * s + 2) * D]
                    gg = modraw[:, (3 * s + 2) * D:(3 * s + 3) * D]
                    P.add("dve", lambda e, s=s, sc=sc: e.scalar_tensor_tensor(out=outv[:, 3 * s + 0, :], in0=sc, scalar=1.0,
                                                                              in1=npre[:, s * D:(s + 1) * D], op0=ALU.add, op1=ALU.mult),
                          reads=["modraw", "npre"], writes=["outv"])
                    P.add("dve", lambda e, s=s, sh=sh: e.tensor_copy(out=outv[:, 3 * s + 1, :], in_=sh),
                          reads=["modraw"], writes=["outv"])
                    P.add("dve", lambda e, s=s, gg=gg: e.tensor_tensor(out=outv[:, 3 * s + 2, :], in0=gg,
                                                                       in1=npost[:, s * D:(s + 1) * D], op=ALU.mult),
                          reads=["modraw", "npost"], writes=["outv"])
                dma("sp", modv[i].rearrange("s q r d -> r (s q) d"), outv[:], ["outv"], ["modv"])
            P.flush()

    def phase_B():
        with ExitStack() as es:
            def sb(name, shape, dt):
                return es.enter_context(nc.sbuf_tensor(name, shape, dt))
            w_in = sb("w_in", [128, 8, DIN], BF16)
            modt = [sb("modB%d" % i, [128, D], F32) for i in range(4)]
            xin = sb("xinB", [128, 2, D], F32)
            tmpsB = [sb("tmpB", [128, D], F32), sb("tmpB2", [128, D], F32)]
            junk = sb("junkB", [128, D], BF16)
            st = sb("stB", [128, 16], F32)
            hn = sb("hnB", [128, 2, D], BF16)
            hT = sb("hTB", [128, 2, 8, 512], BF16)
            cacc = sb("cacc", [128, 2, 512], F32)
            cacc2 = sb("cacc2", [128, 512], F32)
            ucp = sb("ucp", [128, 512], F32)
            xbcT = sb("xbcT", [128, 16, 512], BF16)
            szo = sb("szo", [128, 2, DI], BF16)
            xto = sb("xto", [128, 2, DI], BF16)
            bto = sb("bto", [128, 2, 1024], BF16)
            dtt = sb("dtt", [128, 2, 64], F32)
            cw = sb("cw", [128, 32, 5], F32)
            cb = sb("cb", [128, 32], F32)
            dtb = sb("dtb", [128, 64], F32)
            for k in range(8):
                dma("pool", w_in[:, k, :], ssd_w_in[k * 128:(k + 1) * 128, :], [], ["w_in%d" % k])
            for t in range(5):
                dma_slow("sp", cw[:, :, t], ssd_conv_w[t].rearrange("(c p) -> p c", p=128), [], ["cw"])
            dma_slow("sp", cb[:], ssd_conv_b.rearrange("(c p) -> p c", p=128), [], ["cw"])
            dma("sp", dtb[:], ssd_dt_bias[0:1, :].partition_broadcast(128), [], ["dtb"])
            load_mod(0, 0, 0, modt[0], modt[1], None)
            load_mod(0, 0, 1, modt[2], modt[3], None)
            tiles = [(ctx_in, 0, 256, 256, True, T)]
            for j in range(T // 512):
                tiles.append((x_in, j * 512, 512, 64, False, j * 512))
            subc = [0]
            zc_cnt = [0]
            for ti, (src, r0, W, RW, is_ctx, srow) in enumerate(tiles):
                hb_ = ti % 2
                A_t, B_t = (modt[2], modt[3]) if is_ctx else (modt[0], modt[1])
                nsub = W // 128
                def nmB(s):
                    b = subc[0] % 2
                    subc[0] += 1
                    dma("sp", xin[:, b, :], src[r0 + s * 128:r0 + (s + 1) * 128, :], [], ["xin%d" % b])
                    return b, norm_mod(xin[:, b, :], ["xin%d" % b], A_t, B_t, hn[:, b, :], tmpsB[b], junk, st, "B", par=b)
                cur = nmB(0)
                for s in range(nsub):
                    nxt = nmB(s + 1) if s + 1 < nsub else None
                    b, hres = cur
                    transposes_list([hn[:, b, k * 128:(k + 1) * 128] for k in range(8)], 0,
                                    hT[:, hb_, :, s * 128:(s + 1) * 128], [hres], ["hT%d" % hb_])
                    cur = nxt
                for s in range(nsub):
                    b = subc[0] % 2
                    subc[0] += 1
                    rows = slice(srow + s * 128, srow + (s + 1) * 128)
                    if not is_ctx:
                        for zc in range(4):
                            bk = 1 + zc_cnt[0] % 2
                            zc_cnt[0] += 1

                            def mmz(e, bk=bk, s=s, zc=zc, hb_=hb_):
                                for k in range(8):
                                    r = e.matmul(psb(bk), lhsT=hT[:, hb_, k, s * 128:(s + 1) * 128],
                                                 rhs=w_in[:, k, zc * 512:(zc + 1) * 512], start=(k == 0), stop=(k == 7))
                                return r
                            P.add("pe", mmz, reads=["hT%d" % hb_] + ["w_in%d" % k_ for k_ in range(8)], writes=["ps%d" % bk])
                            P.add("act", lambda e, bk=bk, b=b, zc=zc: e.activation(out=szo[:, b, zc * 512:(zc + 1) * 512],
                                                                                   in_=psb(bk), func=AF.Silu),
                                  reads=["ps%d" % bk], writes=["szo%d" % b])
                        dma("act", sz[rows, :], szo[:, b, :], ["szo%d" % b], ["sz_d"])
                    bk = 1 + zc_cnt[0] % 2
                    zc_cnt[0] += 1

                    def mmdt(e, bk=bk, s=s, hb_=hb_):
                        for k in range(8):
                            r = e.matmul(psb(bk)[:, 0:64], lhsT=hT[:, hb_, k, s * 128:(s + 1) * 128],
                                         rhs=w_in[:, k, 6144:6208], start=(k == 0), stop=(k == 7))
                        return r
                    P.add("pe", mmdt, reads=["hT%d" % hb_] + ["w_in%d" % k_ for k_ in range(8)], writes=["ps%d" % bk])
                    P.add("dve", lambda e, bk=bk, b=b: e.tensor_tensor(out=dtt[:, b, :], in0=psb(bk)[:, 0:64], in1=dtb[:], op=ALU.add),
                          reads=["ps%d" % bk, "dtb"], writes=["dtt%d" % b])
                    P.add("act", lambda e, b=b: e.activation(out=dtt[:, b, :], in_=dtt[:, b, :], func=AF.Exp),
                          reads=["dtt%d" % b], writes=["dtt%d" % b])
                    P.add("act", lambda e, b=b: e.activation(out=dtt[:, b, :], in_=dtt[:, b, :], func=AF.Ln, bias=1.0),
                          reads=["dtt%d" % b], writes=["dtt%d" % b])
                    dma("act", dts[rows, :], dtt[:, b, :], ["dtt%d" % b], ["dts_d"])
                ncc = 24 if is_ctx else 32
                for cc in range(ncc):
                    bk = 3 + cc % 2
                    slot = cc % 16

                    def mmx(e, bk=bk, cc=cc, hb_=hb_, W=W):
                        for k in range(8):
                            r = e.matmul(psb(bk)[:, 0:W], lhsT=w_in[:, k, 2048 + cc * 128:2048 + (cc + 1) * 128],
                                         rhs=hT[:, hb_, k, 0:W], start=(k == 0), stop=(k == 7))
                        return r
                    P.add("pe", mmx, reads=["hT%d" % hb_] + ["w_in%d" % k_ for k_ in range(8)], writes=["ps%d" % bk])

                    on_pool = False
                    if on_pool:
                        P.add("act", lambda e, bk=bk, W=W: e.activation(out=ucp[:, 0:W], in_=psb(bk)[:, 0:W], func=AF.Copy),
                              reads=["ps%d" % bk], writes=["ucp"])
                        ca_v = cacc2[:, 0:W].rearrange("p (r w) -> p r w", w=RW)
                        u_v = ucp[:, 0:W].rearrange("p (r w) -> p r w", w=RW)
                        ceng, crd, cwr, csrc = "pool", ["ucp", "cw"], ["cacc2"], cacc2[:, 0:W]
                        fns = [lambda e, ca_v=ca_v, u_v=u_v, cc=cc: e.tensor_scalar(out=ca_v, in0=u_v, scalar1=cw[:, cc, 2:3], scalar2=0.0,
                                                                                  op0=ALU.mult, op1=ALU.add)]
                    else:
                        ca_v = cacc[:, cc % 2, 0:W].rearrange("p (r w) -> p r w", w=RW)
                        u_v = psb(bk)[:, 0:W].rearrange("p (r w) -> p r w", w=RW)
                        ceng, crd, cwr, csrc = "dve", ["ps%d" % bk, "cw"], ["cacc%d" % (cc % 2)], cacc[:, cc % 2, 0:W]
                        fns = [lambda e, ca_v=ca_v, u_v=u_v, cc=cc: e.tensor_scalar(out=ca_v, in0=u_v, scalar1=cw[:, cc, 2:3], scalar2=None, op0=ALU.mult)]
                    for t in (1, 3, 0, 4):
                        d_ = t - 2
                        if d_ < 0:
                            o = ca_v[:, :, -d_:RW]
                            i0 = u_v[:, :, 0:RW + d_]
                        else:
                            o = ca_v[:, :, 0:RW - d_]
                            i0 = u_v[:, :, d_:RW]
                        fns.append(lambda e, o=o, i0=i0, cc=cc, t=t: e.scalar_tensor_tensor(out=o, in0=i0, scalar=cw[:, cc, t:t + 1], in1=o,
                                                                                         op0=ALU.mult, op1=ALU.add))
                    seq(ceng, fns, crd, cwr)
                    P.add("act", lambda e, cc=cc, slot=slot, W=W, csrc=csrc: e.activation(out=xbcT[:, slot, 0:W], in_=csrc,
                                                                                          func=AF.Silu, bias=cb[:, cc:cc + 1]),
                          reads=cwr + ["cw"], writes=["xbcT%d" % slot])
                    if cc == 15:
                        for s in range(nsub):
                            b = subc[0] % 2
                            subc[0] += 1
                            rows = slice(srow + s * 128, srow + (s + 1) * 128)
                            transposes_list([xbcT[:, k, s * 128:(s + 1) * 128] for k in range(16)], 5,
                                            xto[:, b, :].rearrange("p (k t) -> p k t", t=128),
                                            ["xbcT%d" % k for k in range(16)], ["xto%d" % b])
                            dma("act", x_tok[rows, :], xto[:, b, :], ["xto%d" % b], ["x_tok_d"])
                    if cc == 23:
                        if not is_ctx:
                            dma("act", BT.rearrange("(g n) t -> n g t", n=128)[:, :, srow:srow + W], xbcT[:, 0:8, 0:W],
                                ["xbcT%d" % k for k in range(8)], ["BT_d"])
                        for s in range(nsub):
                            b = subc[0] % 2
                            subc[0] += 1
                            rows = slice(srow + s * 128, srow + (s + 1) * 128)
                            transposes_list([xbcT[:, k, s * 128:(s + 1) * 128] for k in range(8)], 5,
                                            bto[:, b, :].rearrange("p (k t) -> p k t", t=128),
                                            ["xbcT%d" % k for k in range(8)], ["bto%d" % b])
                            dma("act", B_tok[rows, :], bto[:, b, :], ["bto%d" % b], ["B_tok_d"])
                    if cc == 31:
                        dma("act", CT.rearrange("(g n) t -> n g t", n=128)[:, :, srow:srow + W], xbcT[:, 8:16, 0:W],
                            ["xbcT%d" % k for k in range(8, 16)], ["CT_d"])
            P.flush()

    def phase_C():
        with ExitStack() as es:
            def sb(name, shape, dt):
                return es.enter_context(nc.sbuf_tensor(name, shape, dt))
            Lex, Uex, ones, A_b = tri_consts(es)
            xt = sb("xtC", [128, 2, DI], BF16)
            bt = sb("btC", [128, 2, 1024], BF16)
            dtc = sb("dtcC", [128, 2, 64], F32)
            dA = sb("dAC", [128, 64], F32)
            exs = sb("exsC", [128, 128], F32)
            cf = sb("cfC", [128, 64], F32)
            xdtd = sb("xdtdC", [128, DI], BF16)
            hst = [sb("hFC", [128, DI], F32), sb("hBC", [128, DI], F32)]
            hbo = sb("hboC", [128, 2, DI], BF16)
            for d_ in range(2):
                P.add("pool", lambda e, d_=d_: e.memset(hst[d_][:], 0.0), writes=["h%d" % d_])
            cnt = [0]

            def step(row0, d_, store=None):
                b = cnt[0] % 2
                cnt[0] += 1
                rows = slice(row0, row0 + 128)
                dma("sp", xt[:, b, :], x_tok[rows, :], ["x_tok_d"], ["xt%d" % b])
                dma("sp", bt[:, b, :], B_tok[rows, :], ["B_tok_d"], ["bt%d" % b])
                dma("sp", dtc[:, b, :], dts[rows, :], ["dts_d"], ["dtc%d" % b])
                P.add("dve", lambda e: e.tensor_tensor(out=dA[:], in0=dtc[:, b, :], in1=A_b[:], op=ALU.mult),
                      reads=["dtc%d" % b, "A_b"], writes=["dA"])
                sl = slice(d_ * 32, (d_ + 1) * 32)
                tri = Uex if d_ == 0 else Lex

                def mms(e):
                    e.matmul(ps[:, 0, 0:32], lhsT=tri[:], rhs=dA[:, sl], start=True, stop=True)
                    return e.matmul(ps[:, 0, 32:64], lhsT=ones[:], rhs=dA[:, sl], start=True, stop=True)
                P.add("pe", mms, reads=["dA", "tri"], writes=["ps0"])
                P.add("act", lambda e: e.activation(out=exs[:, 0:64], in_=ps[:, 0, 0:64], func=AF.Exp),
                      reads=["ps0"], writes=["exs"])
                P.add("dve", lambda e: e.tensor_tensor(out=cf[:, 0:32], in0=dtc[:, b, sl], in1=exs[:, 0:32], op=ALU.mult),
                      reads=["dtc%d" % b, "exs"], writes=["cf"])
                P.add("dve", lambda e: e.tensor_tensor(out=bview(xdtd[:], 32, 64), in0=bview(xt[:, b, :], 32, 64),
                                                       in1=cf[:, 0:32].unsqueeze(2).to_broadcast([128, 32, 64]), op=ALU.mult),
                      reads=["xt%d" % b, "cf"], writes=["xdtd"])

                def mmst(e):
                    for g in range(8):
                        r = e.matmul(ps[:, 4 + g // 2, (g % 2) * 256:(g % 2 + 1) * 256], lhsT=bt[:, b, g * 128:(g + 1) * 128],
                                     rhs=xdtd[:, g * 256:(g + 1) * 256], start=True, stop=True)
                    return r
                P.add("pe", mmst, reads=["bt%d" % b, "xdtd"], writes=["ps4", "ps5", "ps6", "ps7"])
                h = hst[d_]
                if store is not None:
                    ob = store % 2
                    P.add("act", lambda e: e.activation(out=hbo[:, ob, :], in_=h[:], func=AF.Copy),
                          reads=["h%d" % d_], writes=["hbo%d" % ob])
                    dma("sp", hb[store], hbo[:, ob, :], ["hbo%d" % ob], ["hb_d"])
                P.add("dve", lambda e: e.tensor_tensor(out=bview(h[:], 32, 64), in0=bview(h[:], 32, 64),
                                                       in1=exs[:, 32:64].unsqueeze(2).to_broadcast([128, 32, 64]), op=ALU.mult),
                      reads=["exs", "h%d" % d_], writes=["h%d" % d_])
                P.add("dve", lambda e: e.tensor_tensor(out=h[:], in0=ps[:, 4:8, :].rearrange("p b n -> p (b n)"), in1=h[:], op=ALU.add),
                      reads=["ps4", "ps5", "ps6", "ps7", "h%d" % d_], writes=["h%d" % d_])

            step(T, 0)
            step(T + 128, 0)
            step(T + 128, 1)
            step(T, 1)
            for c in range(NCH - 1, -1, -1):
                step(c * 128, 1, store=c)
            dma("sp", hf0[:, :], hst[0][:], ["h0"], ["hf0_d"])
            P.flush()

    def phase_D():
        with ExitStack() as es:
            def sb(name, shape, dt):
                return es.enter_context(nc.sbuf_tensor(name, shape, dt))
            Lex, Uex, ones, A_b = tri_consts(es)
            negF = sb("negF", [128, 4, 128], BF16)
            negB = sb("negB", [128, 4, 128], BF16)
            D_b = sb("D_b", [128, 32], F32)
            DI = sb("DID", [128, 32, 128], F32)
            nw = sb("nwD", [128, 2048], F32)
            G1 = sb("G1D", [128, D], F32)
            w_out = sb("w_outD", [128, 16, D], BF16)
            xt = sb("xtD", [128, 2, 2048], BF16)
            btk = sb("btkD", [128, 2, 1024], BF16)
            BTc = sb("BTcD", [128, 2, 8, 128], BF16)
            CTc = sb("CTcD", [128, 2, 8, 128], BF16)
            szc = sb("szcD", [128, 2, 2048], BF16)
            dtc = sb("dtcD", [128, 2, 64], F32)
            hbc = sb("hbcD", [128, 2048], BF16)
            xres = sb("xresD", [128, 2, D], F32)
            dA = sb("dAD", [128, 64], F32)
            lndt = sb("lndtD", [128, 64], F32)
            acum = sb("acumD", [128, 2, 64], F32)
            nacum = sb("nacumD", [128, 2, 64], F32)
            ea = sb("eaD", [128, 64], F32)
            exs = sb("exsD", [128, 64], F32)
            cf = sb("cfD", [128, 32], F32)
            xdtd = sb("xdtdD", [128, 2048], BF16)
            hF = sb("hFD", [128, 2048], F32)
            hFb = sb("hFbD", [128, 2048], BF16)
            cbs = sb("cbsD", [128, 2, 8, 128], F32)
            ET = sb("ETD", [128, 4, 512], F32)
            MT = sb("MTD", [128, 2, 2, 512], BF16)
            MF = sb("MFD", [128, 2, 512], F32)
            ty = sb("tyD", [128, 2, 2048], F32)
            tyb = sb("tybD", [128, 2048], F32)
            ysb = sb("ysbD", [128, 2, 2048], F32)
            sq = sb("sqD", [128, 2048], F32)
            yn = sb("ynD", [128, 2048], BF16)
            ynT = sb("ynTD", [128, 16, 128], BF16)
            junk = sb("junkD", [128, D], BF16)
            st = sb("stD", [128, 8], F32)
            gst = sb("gstD", [128, 16], F32)

            fns = [lambda e: e.memset(negF[:], NEG), lambda e: e.memset(negB[:], NEG)]
            for j in range(4):
                fns.append(lambda e, j=j: e.affine_select(out=negF[:, j, :], in_=negF[:, j, :], pattern=[[-1, 128]],
                                                          compare_op=ALU.is_gt, fill=0.0, base=0, channel_multiplier=1))
                fns.append(lambda e, j=j: e.affine_select(out=negB[:, j, :], in_=negB[:, j, :], pattern=[[1, 128]],
                                                          compare_op=ALU.is_gt, fill=0.0, base=0, channel_multiplier=-1))
            seq("pool", fns, [], ["neg"])
            dma("sp", D_b[:], ssd_d[0:1, :].partition_broadcast(128), [], ["D_b"])
            for h in range(32):
                P.add("dve", lambda e, h=h: e.tensor_scalar(out=DI[:, h, :], in0=identf[:], scalar1=D_b[:, h:h + 1], scalar2=None, op0=ALU.mult),
                      reads=["D_b", "ident"], writes=["DI"])
            dma("sp", nw[:], ssd_norm[0:1, :].partition_broadcast(128), [], ["nw"])
            load_mod(0, 0, 0, None, None, G1)
            for k in range(4):
                dma("pool", w_out[:, k * 4:(k + 1) * 4, :],
                    ssd_w_out[k * 512:(k + 1) * 512, :].rearrange("(c p) d -> p c d", p=128), [], ["w_out"])
            dma("sp", hF[:], hf0[:, :], ["hf0_d"], ["hF"])

            def front_loads(c):
                b = c % 2
                rows = slice(c * 128, (c + 1) * 128)
                dma("sp", dtc[:, b, :], dts[rows, :], ["dts_d"], ["dtc%d" % b])
                dma("sp", xt[:, b, :], x_tok[rows, :], ["x_tok_d"], ["xt%d" % b])
                dma("sp", CTc[:, b], CT.rearrange("(g n) t -> n g t", n=128)[:, :, rows], ["CT_d"], ["CTc%d" % b])
                dma("sp", hbc[:], hb[c], ["hb_d"], ["hbc"])
                dma("sp", BTc[:, b], BT.rearrange("(g n) t -> n g t", n=128)[:, :, rows], ["BT_d"], ["BTc%d" % b])
                dma("sp", btk[:, b, :], B_tok[rows, :], ["B_tok_d"], ["btk%d" % b])

            def front(c):
                b = c % 2
                rows = slice(c * 128, (c + 1) * 128)
                dma("sp", szc[:, b, :], sz[rows, :], ["sz_d"], ["szc%d" % b])
                P.add("dve", lambda e: e.tensor_tensor(out=dA[:], in0=dtc[:, b, :], in1=A_b[:], op=ALU.mult),
                      reads=["dtc%d" % b, "A_b"], writes=["dA"])
                P.add("act", lambda e: e.activation(out=lndt[:], in_=dtc[:, b, :], func=AF.Ln), reads=["dtc%d" % b], writes=["lndt"])
                P.add("act", lambda e: e.activation(out=hFb[:], in_=hF[:], func=AF.Copy), reads=["hF"], writes=["hFb"])
                yield

                def mms(e):
                    e.matmul(ps[:, 6, 0:32], lhsT=Lex[:], rhs=dA[:, 0:32], start=True, stop=True)
                    e.matmul(ps[:, 6, 32:64], lhsT=Uex[:], rhs=dA[:, 32:64], start=True, stop=True)
                    e.matmul(ps[:, 6, 64:96], lhsT=Uex[:], rhs=dA[:, 0:32], start=True, stop=True)
                    return e.matmul(ps[:, 6, 96:128], lhsT=ones[:], rhs=dA[:, 0:32], start=True, stop=True)
                P.add("pe", mms, reads=["dA", "tri"], writes=["ps6"])
                yield
                P.add("dve", lambda e: e.tensor_tensor(out=acum[:, b, :], in0=ps[:, 6, 0:64], in1=dA[:], op=ALU.add),
                      reads=["ps6", "dA"], writes=["acum%d" % b])
                P.add("act", lambda e: e.activation(out=exs[:], in_=ps[:, 6, 64:128], func=AF.Exp), reads=["ps6"], writes=["exs"])
                yield
                P.add("dve", lambda e: e.tensor_tensor(out=nacum[:, b, :], in0=lndt[:], in1=acum[:, b, :], op=ALU.subtract),
                      reads=["acum%d" % b, "lndt"], writes=["nacum%d" % b])
                P.add("act", lambda e: e.activation(out=ea[:], in_=acum[:, b, :], func=AF.Exp), reads=["acum%d" % b], writes=["ea"])
                P.add("dve", lambda e: e.tensor_tensor(out=cf[:], in0=dtc[:, b, 0:32], in1=exs[:, 0:32], op=ALU.mult),
                      reads=["dtc%d" % b, "exs"], writes=["cf"])
                yield
                P.add("dve", lambda e: e.tensor_tensor(out=bview(xdtd[:], 32, 64), in0=bview(xt[:, b, :], 32, 64),
                                                       in1=cf[:].unsqueeze(2).to_broadcast([128, 32, 64]), op=ALU.mult),
                      reads=["xt%d" % b, "cf"], writes=["xdtd"])
                yield

                def scale(g):
                    for d_ in range(2):
                        tb = ty[:, b, :] if d_ == 0 else tyb[:]
                        P.add("dve", lambda e, g=g, d_=d_, tb=tb: e.tensor_tensor(
                            out=bview(tb[:, g * 256:(g + 1) * 256], 4, 64),
                            in0=bview(ps[:, 7, d_ * 256:(d_ + 1) * 256], 4, 64),
                            in1=ea[:, d_ * 32 + g * 4:d_ * 32 + (g + 1) * 4].unsqueeze(2).to_broadcast([128, 4, 64]), op=ALU.mult),
                            reads=["ps7", "ea"], writes=["ty%d" % b if d_ == 0 else "tyb"])
                for g in range(8):
                    def mmoff(e, g=g):
                        e.matmul(ps[:, 7, 0:256], lhsT=CTc[:, b, g, :], rhs=hFb[:, g * 256:(g + 1) * 256], start=True, stop=True)
                        return e.matmul(ps[:, 7, 256:512], lhsT=CTc[:, b, g, :], rhs=hbc[:, g * 256:(g + 1) * 256],
                                        start=True, stop=True)
                    P.add("pe", mmoff, reads=["CTc%d" % b, "hFb", "hbc"], writes=["ps7"])
                    yield
                    scale(g)
                yield

                def hupd(gp):
                    cs = slice(gp * 512, (gp + 1) * 512)
                    P.add("dve", lambda e, gp=gp, cs=cs: e.tensor_tensor(
                        out=bview(hF[:, cs], 8, 64), in0=bview(hF[:, cs], 8, 64),
                        in1=exs[:, 32 + gp * 8:32 + (gp + 1) * 8].unsqueeze(2).to_broadcast([128, 8, 64]), op=ALU.mult),
                        reads=["hF", "hFb", "exs"], writes=["hF"])
                    P.add("dve", lambda e, cs=cs: e.tensor_tensor(out=hF[:, cs], in0=ps[:, 6, :], in1=hF[:, cs], op=ALU.add),
                          reads=["ps6", "hF"], writes=["hF"])
                for gp in range(4):
                    def mmst(e, gp=gp):
                        for g in (2 * gp, 2 * gp + 1):
                            r = e.matmul(ps[:, 6, (g % 2) * 256:(g % 2 + 1) * 256], lhsT=btk[:, b, g * 128:(g + 1) * 128],
                                         rhs=xdtd[:, g * 256:(g + 1) * 256], start=True, stop=True)
                        return r
                    P.add("pe", mmst, reads=["btk%d" % b, "xdtd"], writes=["ps6"])
                    yield
                    hupd(gp)
                P.add("pool", lambda e: e.tensor_tensor(out=ty[:, b, :], in0=ty[:, b, :], in1=tyb[:], op=ALU.add),
                      reads=["ty%d" % b, "tyb"], writes=["ty%d" % b])
                yield

                def mmcb(e):
                    for g in range(8):
                        r = e.matmul(ps[:, g // 4, (g % 4) * 128:(g % 4 + 1) * 128], lhsT=BTc[:, b, g, :], rhs=CTc[:, b, g, :],
                                     start=True, stop=True)
                    return r
                P.add("pe", mmcb, reads=["BTc%d" % b, "CTc%d" % b], writes=["ps0", "ps1"])
                yield
                P.add("act", lambda e: e.activation(out=cbs[:, b].rearrange("p g t -> p (g t)"),
                                                    in_=ps[:, 0:2, :].rearrange("p b n -> p (b n)"), func=AF.Copy),
                      reads=["ps0", "ps1"], writes=["cbs%d" % b])
                yield

            rc = [0]

            def mid(c, side, nside):
                b = c % 2

                def diag(g):
                    yb = 4 + (g // 2) % 2

                    def mmdiag(e, g=g, yb=yb):
                        first = (g % 2 == 0)
                        for d_ in range(2):
                            for j in range(4):
                                hh = g * 4 + j
                                c0 = (g % 2) * 256 + j * 64
                                r = e.matmul(ps[:, yb, c0:c0 + 64], lhsT=MT[:, g % 2, d_, j * 128:(j + 1) * 128],
                                             rhs=xt[:, b, hh * 64:(hh + 1) * 64],
                                             start=(first and d_ == 0 and j == 0), stop=(g % 2 == 1 and d_ == 1 and j == 3))
                        return r
                    P.add("pe", mmdiag, reads=["MT%d0" % (g % 2), "MT%d1" % (g % 2), "xt%d" % b], writes=["ps%d" % yb])
                    if g % 2 == 1:
                        cs = slice((g - 1) * 256, (g + 1) * 256)
                        P.add("dve", lambda e, yb=yb, cs=cs: e.tensor_tensor(out=ysb[:, b, cs], in0=ps[:, yb, :], in1=ty[:, b, cs], op=ALU.add),
                              reads=["ps%d" % yb, "ty%d" % b], writes=["ysb%d" % b])

                for g in range(8):
                    for d_ in range(2):
                        rb = 2 + rc[0] % 2
                        eb = rc[0] % 4
                        rc[0] += 1
                        neg = negF if d_ == 0 else negB

                        def mmR(e, g=g, d_=d_, rb=rb, neg=neg):
                            e.matmul(ps[:, rb, :], lhsT=identb[:], rhs=neg[:].rearrange("p j t -> p (j t)"), start=True, stop=False)
                            for j in range(4):
                                h = d_ * 32 + g * 4 + j
                                r = e.matmul(ps[:, rb, j * 128:(j + 1) * 128], lhsT=acum[:, b, h:h + 1].to_broadcast([128, 128]),
                                             rhs=identf[:], start=False, stop=(j == 3))
                            return r
                        P.add("pe", mmR, reads=["acum%d" % b, "neg", "ident"], writes=["ps%d" % rb])

                        def exps(e, g=g, d_=d_, rb=rb, eb=eb):
                            for j in range(4):
                                h = d_ * 32 + g * 4 + j
                                r = e.activation(out=ET[:, eb, j * 128:(j + 1) * 128], in_=ps[:, rb, j * 128:(j + 1) * 128],
                                                 func=AF.Exp, bias=nacum[:, b, h:h + 1])
                            return r
                        P.add("act", exps, reads=["ps%d" % rb, "nacum%d" % b], writes=["ET%d" % eb])
                        if d_ == 0:
                            P.add("dve", lambda e, g=g, eb=eb: e.tensor_tensor(
                                out=MF[:, g % 2, :].rearrange("p (j t) -> p j t", j=4),
                                in0=ET[:, eb, :].rearrange("p (j t) -> p j t", j=4),
                                in1=cbs[:, b, g, :].unsqueeze(1).to_broadcast([128, 4, 128]), op=ALU.mult),
                                reads=["ET%d" % eb, "cbs%d" % b], writes=["MF%d" % (g % 2)])
                            P.add("dve", lambda e, g=g: e.tensor_tensor(
                                out=MT[:, g % 2, 0, :], in0=MF[:, g % 2, :],
                                in1=DI[:, g * 4:(g + 1) * 4, :].rearrange("p j t -> p (j t)"), op=ALU.add),
                                reads=["MF%d" % (g % 2), "DI"], writes=["MT%d0" % (g % 2)])
                        else:
                            P.add("dve", lambda e, g=g, eb=eb: e.tensor_tensor(
                                out=MT[:, g % 2, 1, :].rearrange("p (j t) -> p j t", j=4),
                                in0=ET[:, eb, :].rearrange("p (j t) -> p j t", j=4),
                                in1=cbs[:, b, g, :].unsqueeze(1).to_broadcast([128, 4, 128]), op=ALU.mult),
                                reads=["ET%d" % eb, "cbs%d" % b], writes=["MT%d1" % (g % 2)])
                    if g >= 1:
                        diag(g - 1)
                    for _ in range(nside):
                        next(side, None)
                diag(7)
                for _ in side:
                    pass

            def finA(c):
                b = c % 2
                yv = ysb[:, b, :]
                yr = "ysb%d" % b
                P.add("dve", lambda e: e.tensor_tensor(out=yv, in0=yv, in1=szc[:, b, :], op=ALU.mult),
                      reads=[yr, "szc%d" % b], writes=[yr])
                yield
                P.add("act", lambda e: e.activation(out=sq[:], in_=yv, func=AF.Square), reads=[yr, "Dtmp"], writes=["Dtmp"])
                yield
                P.add("dve", lambda e: e.tensor_reduce(out=gst[:, 0:8], in_=bview(sq[:], 8, 256), axis=mybir.AxisListType.X, op=ALU.add),
                      reads=["Dtmp"], writes=["gst"])
                yield
                P.add("act", lambda e: e.activation(out=gst[:, 8:16], in_=gst[:, 0:8], func=AF.Sqrt, bias=EPS, scale=1.0 / 256),
                      reads=["gst"], writes=["gst2"])
                yield
                P.add("dve", lambda e: e.reciprocal(out=gst[:, 8:16], in_=gst[:, 8:16]), reads=["gst2"], writes=["gst2"])
                P.add("dve", lambda e: e.tensor_tensor(out=bview(yv, 8, 256), in0=bview(yv, 8, 256),
                                                       in1=gst[:, 8:16].unsqueeze(2).to_broadcast([128, 8, 256]), op=ALU.mult),
                      reads=["gst2", yr], writes=[yr])
                yield
                P.add("pool", lambda e: e.tensor_tensor(out=yn[:], in0=yv, in1=nw[:], op=ALU.mult),
                      reads=[yr, "nw"], writes=["yn"])
                yield

            def finB(c):
                b = c % 2
                rows = slice(c * 128, (c + 1) * 128)
                dma("sp", xres[:, b, :], x_in[rows, :], [], ["xres%d" % b])

                def tr(e):
                    for k in range(16):
                        r = e.transpose(out=psb16(k // 8)[:, (k % 8) * 128:(k % 8 + 1) * 128], in_=yn[:, k * 128:(k + 1) * 128],
                                        identity=identb[:])
                    return r
                P.add("pe", tr, reads=["yn", "ident"], writes=["ps0", "ps1"])
                yield
                for i in range(2):
                    P.add("act", lambda e, i=i: e.activation(out=ynT[:, i * 8:(i + 1) * 8, :],
                                                             in_=psb16(i).rearrange("p (k t) -> p k t", t=128), func=AF.Copy),
                          reads=["ps%d" % i], writes=["ynT"])
                yield

                def mmo(e):
                    for half in range(2):
                        for k in range(16):
                            r = e.matmul(ps[:, 6 + half, :], lhsT=ynT[:, k, :], rhs=w_out[:, k, half * 512:(half + 1) * 512],
                                         start=(k == 0), stop=(k == 15))
                    return r
                P.add("pe", mmo, reads=["ynT", "w_out"], writes=["ps6", "ps7"])
                yield
                y_ap = ps[:, 6:8, :].rearrange("p b n -> p (b n)")
                P.add("act", lambda e: e.activation(out=junk[:], in_=y_ap, func=AF.Square, accum_out=st[:, 2:3]),
                      reads=["ps6", "ps7"], writes=["Dpjunk", "Dpss"])
                yield
                P.add("act", lambda e: e.activation(out=st[:, 3:4], in_=st[:, 2:3], func=AF.Sqrt, bias=EPS, scale=1.0 / D),
                      reads=["Dpss"], writes=["Dprs"])
                yield
                P.add("dve", lambda e: e.reciprocal(out=st[:, 3:4], in_=st[:, 3:4]), reads=["Dprs"], writes=["Dprs"])
                P.add("dve", lambda e: e.scalar_tensor_tensor(out=sq[:, 0:D], in0=y_ap, scalar=st[:, 3:4], in1=G1[:],
                                                              op0=ALU.mult, op1=ALU.mult),
                      reads=["ps6", "ps7", "Dprs", "modtiles"], writes=["Dtmp"])
                yield
                P.add("pool", lambda e: e.tensor_tensor(out=xres[:, b, :], in0=xres[:, b, :], in1=sq[:, 0:D], op=ALU.add),
                      reads=["Dtmp", "xres%d" % b], writes=["xres%d" % b])
                yield
                dma("sp", x1[rows, :], xres[:, b, :], ["xres%d" % b], ["x1_d"])
                yield

            import itertools
            front_loads(0)
            if d_stop >= 1:
                for _ in front(0):
                    pass
            for c in range(NCH if d_stop >= 2 else 0):
                if d_stop == 2 and c >= 1:
                    break
                if d_stop == 3 and c >= 2:
                    break
                if c + 1 < NCH:
                    front_loads(c + 1)
                gens = []
                if c >= 1:
                    gens += [finA(c - 1), finB(c - 1)]
                if c + 1 < NCH:
                    gens.append(front(c + 1))
                mid(c, itertools.chain(*gens), NSIDE)
            if d_stop >= 4:
                for _ in itertools.chain(finA(NCH - 1), finB(NCH - 1)):
                    pass
            P.flush()

    def bdm(bd, half):
        return bd + half

    I32 = mybir.dt.int32
    hs = dt_("hs", [NE * T, 512], F32).ap()
    ys = dt_("ys", [NE * T, D], F32).ap()
    x2 = dt_("x2", [T, D], F32).ap()
    NI = T // 512
    ctab = dt_("ctab", [1, NE * NI], I32).ap()

    def phase_E():
        NT = T // 1024
        NS = T // 128
        with ExitStack() as es0:
            def sb0(name, shape, dt):
                return es0.enter_context(nc.sbuf_tensor(name, shape, dt))
            idx = [[sb0("idx%d_%d" % (k, n), [128, 1], I32) for n in range(NS)] for k in range(2)]
            gts = sb0("gtsE", [128, NS, 2], F32)
            carry = sb0("carryE", [128, NE], F32)
            P.add("pool", lambda e: e.memset(carry[:], 0.0), writes=["carry"])

            with ExitStack() as es:
                def sb(name, shape, dt):
                    return es.enter_context(nc.sbuf_tensor(name, shape, dt))
                xres = sb("xresE", [128, 8, D], F32)
                hT = sb("hTE", [128, 8, 1024], BF16)
                acc = sb("accE", [128, 8, D], F32)
                aT = sb("aTE", [128, 2, 4, 1024], BF16)
                wgu = sb("wguE", [128, 2, 8, 2, 512], BF16)
                wdn = sb("wdnE", [128, 2, 4, D], BF16)
                At = sb("AtE", [128, D], F32)
                Bt = sb("BtE", [128, D], F32)
                Gt = sb("GtE", [128, D], F32)
                tmp = sb("tmpE", [128, D], F32)
                tmpb = sb("tmpbE", [128, D], F32)
                tmps = [tmp, tmpb]
                junk = sb("junkE", [128, D], F32)
                st = sb("stE", [128, 16], F32)
                hn = sb("hnE", [128, 2, D], BF16)
                hn32 = sb("hn32E", [128, D], F32)
                stmp = sb("stmpE", [128, 2, 512], F32)
                uu = sb("uuE", [128, 2, 512], F32)
                ca = sb("caE", [128, 2, 512], F32)
                scw3 = sb("scw3E", [128, 8, 3], F32)
                h32T = sb("h32TE", [128, 8, 128], F32)
                rt = sb("rtE", [128, 8, NE], F32)
                lg = sb("lgE", [128, 6, NE], F32)
                sm = sb("smE", [128, 12], F32)
                Lex = sb("LexE", [128, 128], F32)
                ones = sb("onesE", [128, 128], F32)
                ebase = sb("ebaseE", [128, NE], F32)
                seq("pool", [
                    lambda e: e.memset(ones[:], 1.0),
                    lambda e: e.memset(Lex[:], 1.0),
                    lambda e: e.affine_select(out=Lex[:], in_=Lex[:], pattern=[[1, 128]], compare_op=ALU.is_gt, fill=0.0,
                                              base=0, channel_multiplier=-1),
                ], [], ["tri"])
                for ex in range(NE):
                    P.add("pool", lambda e, ex=ex: e.memset(ebase[:, ex:ex + 1], float(ex * T)), writes=["ebase"])
                for t in range(3):
                    dma_slow("sp", scw3[:, :, t], sc_conv_w[t].rearrange("(c p) -> p c", p=128), [], ["scw3"])
                dma("sp", rt[:], moe_router.rearrange("(k p) e -> p k e", p=128), [], ["rt"])
                cnt = dict(blk=0, gu=0, dn=0, sc=0)

                pending = [None]

                def ffn_dense():
                    F = DFF
                    nfc = F // 128
                    wv = ffn_w_gu.rearrange("(k p) n -> p k n", p=128)
                    fc0 = 0
                    bi = 0
                    while fc0 < nfc:
                        nb = min(4, nfc - fc0)
                        b = cnt["blk"] % 2
                        cnt["blk"] += 1
                        dma("pool", wgu[:, b, :, 0, 0:nb * 128], wv[:, :, fc0 * 128:(fc0 + nb) * 128], [], ["wgug%d" % b])
                        dma("pool", wgu[:, b, :, 1, 0:nb * 128], wv[:, :, F + fc0 * 128:F + (fc0 + nb) * 128], [], ["wguu%d" % b])
                        dma("pool", wdn[:, b, 0:nb, :], ffn_w_down[fc0 * 128:(fc0 + nb) * 128, :].rearrange("(c p) d -> p c d", p=128),
                            [], ["wdn%d" % b])
                        for j in range(nb):
                            for th in range(2):
                                q = cnt["gu"] % 2
                                cnt["gu"] += 1
                                bg = q * 2
                                bu = q * 2 + 1

                                def mmgu(e, b=b, j=j, th=th, bg=bg, bu=bu):
                                    for k in range(8):
                                        e.matmul(psb(bg), lhsT=wgu[:, b, k, 0, j * 128:(j + 1) * 128], rhs=hT[:, k, th * 512:(th + 1) * 512],
                                                 start=(k == 0), stop=(k == 7))
                                    for k in range(8):
                                        r = e.matmul(psb(bu), lhsT=wgu[:, b, k, 1, j * 128:(j + 1) * 128], rhs=hT[:, k, th * 512:(th + 1) * 512],
                                                     start=(k == 0), stop=(k == 7))
                                    return r
                                P.add("pe", mmgu, reads=["wgug%d" % b, "wguu%d" % b, "hT"], writes=["ps%d" % bg, "ps%d" % bu])
                                P.add("act", lambda e, q=q, bg=bg: e.activation(out=stmp[:, q, :], in_=psb(bg), func=AF.Silu),
                                      reads=["ps%d" % bg], writes=["stmp%d" % q])
                                P.add("dve", lambda e, q=q, bu=bu, b=b, j=j, th=th: e.tensor_tensor(
                                    out=aT[:, b, j, th * 512:(th + 1) * 512], in0=psb(bu), in1=stmp[:, q, :], op=ALU.mult),
                                    reads=["ps%d" % bu, "stmp%d" % q], writes=["aT%d" % b])
                        def down(b=b, nb=nb, bi=bi):
                            for s in range(8):
                                q = cnt["dn"] % 2
                                cnt["dn"] += 1
                                bd = 4 + q * 2

                                def mmdn(e, b=b, s=s, bd=bd, nb=nb):
                                    for half in range(2):
                                        for j in range(nb):
                                            r = e.matmul(psb(bd + half), lhsT=aT[:, b, j, s * 128:(s + 1) * 128],
                                                         rhs=wdn[:, b, j, half * 512:(half + 1) * 512], start=(j == 0), stop=(j == nb - 1))
                                    return r
                                P.add("pe", mmdn, reads=["aT%d" % b, "wdn%d" % b], writes=["ps%d" % bd, "ps%d" % (bd + 1)])
                                srcp = ps[:, bd:bd + 2, :].rearrange("p b n -> p (b n)")
                                rd = ["ps%d" % bd, "ps%d" % (bd + 1)]
                                if bi == 0:
                                    P.add("dve", lambda e, s=s, srcp=srcp: e.tensor_copy(out=acc[:, s, :], in_=srcp),
                                          reads=rd, writes=["acc%d" % s])
                                else:
                                    P.add("dve", lambda e, s=s, srcp=srcp: e.tensor_tensor(out=acc[:, s, :], in0=srcp, in1=acc[:, s, :], op=ALU.add),
                                          reads=rd + ["acc%d" % s], writes=["acc%d" % s])
                        if pending[0] is not None:
                            pending[0]()
                        pending[0] = down
                        fc0 += nb
                        bi += 1
                    pending[0]()
                    pending[0] = None

                XA = mybir.AxisListType.X

                def norm_phase():
                    def nm(s):
                        return norm_mod(xres[:, s, :], ["xres%d" % s], At, Bt, hn[:, s % 2, :], tmps[s % 2], junk, st, "E", par=s % 2)
                    hres = nm(0)
                    for s in range(8):
                        nxt = nm(s + 1) if s + 1 < 8 else None
                        transposes_list([hn[:, s % 2, k * 128:(k + 1) * 128] for k in range(8)], 7,
                                        hT[:, :, s * 128:(s + 1) * 128], [hres], ["hT"])
                        hres = nxt

                for tt in range(NT):
                    t0 = tt * 1024
                    for s in range(8):
                        dma("sp", xres[:, s, :], x1[t0 + s * 128:t0 + (s + 1) * 128, :], ["x1_d"], ["xres%d" % s])
                    load_mod(0, 1, 0, At, Bt, Gt)
                    norm_phase()
                    ffn_dense()
                    for s in range(8):
                        post_norm_res(acc[:, s, :], ["acc%d" % s], Gt, xres[:, s, :], ["xres%d" % s], tmps[s % 2], junk, st, "E", par=s % 2)
                    if dbg:
                        for s in range(8):
                            dma("sp", dbg_ffn[t0 + s * 128:t0 + (s + 1) * 128, :], xres[:, s, :], ["xres%d" % s], ["dbg_ffn"])
                    load_mod(1, 0, 0, At, Bt, Gt)
                    norm_phase()
                    for k2 in range(2):
                        dma("pool", wdn[:, k2, :, :], sc_w_out[k2 * 512:(k2 + 1) * 512, :].rearrange("(c p) d -> p c d", p=128),
                            [], ["wdn%d" % k2])
                    scv = sc_w_in.rearrange("(k p) (j c) -> p k j c", p=128, j=3)
                    for i in range(8):
                        b = cnt["blk"] % 2
                        cnt["blk"] += 1
                        scw = wgu[:, b].rearrange("p k j c -> p (k j c)")[:, 0:3072].rearrange("p (k j c) -> p k j c", k=8, j=3)
                        for j3 in range(3):
                            dma("pool", scw[:, :, j3, :], scv[:, :, j3, i * 128:(i + 1) * 128], [], ["wgug%d" % b, "wguu%d" % b])
                        for th in range(2):
                            q = cnt["sc"] % 2
                            cnt["sc"] += 1
                            B0 = q * 4

                            def mmsc(e, scw=scw, th=th, B0=B0):
                                for j in range(3):
                                    for k in range(8):
                                        r = e.matmul(psb(B0 + j), lhsT=scw[:, k, j, :], rhs=hT[:, k, th * 512:(th + 1) * 512],
                                                     start=(k == 0), stop=(k == 7))
                                return r
                            P.add("pe", mmsc, reads=["wgug%d" % b, "wguu%d" % b, "hT"], writes=["ps%d" % (B0 + j) for j in range(3)])
                            P.add("act", lambda e, q=q, B0=B0: e.activation(out=stmp[:, q, :], in_=psb(B0 + 2), func=AF.Copy),
                                  reads=["ps%d" % (B0 + 2)], writes=["stmp%d" % q])
                            P.add("dve", lambda e, q=q, B0=B0: e.tensor_tensor(out=uu[:, q, :], in0=psb(B0 + 1), in1=stmp[:, q, :], op=ALU.mult),
                                  reads=["ps%d" % (B0 + 1), "stmp%d" % q], writes=["uu%d" % q])
                            cv = ca[:, q, :].rearrange("p (r w) -> p r w", w=64)
                            uv = uu[:, q, :].rearrange("p (r w) -> p r w", w=64)
                            seq("dve", [
                                lambda e, cv=cv, uv=uv, i=i: e.tensor_scalar(out=cv, in0=uv, scalar1=scw3[:, i, 1:2], scalar2=None, op0=ALU.mult),
                                lambda e, cv=cv, uv=uv, i=i: e.scalar_tensor_tensor(out=cv[:, :, 1:64], in0=uv[:, :, 0:63], scalar=scw3[:, i, 0:1],
                                                                                  in1=cv[:, :, 1:64], op0=ALU.mult, op1=ALU.add),
                                lambda e, cv=cv, uv=uv, i=i: e.scalar_tensor_tensor(out=cv[:, :, 0:63], in0=uv[:, :, 1:64], scalar=scw3[:, i, 2:3],
                                                                                  in1=cv[:, :, 0:63], op0=ALU.mult, op1=ALU.add),
                            ], ["uu%d" % q, "scw3"], ["ca%d" % q])
                            P.add("dve", lambda e, q=q, B0=B0, i=i, th=th: e.tensor_tensor(
                                out=aT[:, i // 4, i % 4, th * 512:(th + 1) * 512], in0=psb(B0), in1=ca[:, q, :], op=ALU.mult),
                                reads=["ps%d" % B0, "ca%d" % q], writes=["aT%d" % (i // 4)])
                    for s in range(8):
                        q = cnt["dn"] % 2
                        cnt["dn"] += 1
                        bd = 4 + q * 2

                        def mmso(e, s=s, bd=bd):
                            for half in range(2):
                                for i in range(8):
                                    r = e.matmul(psb(bd + half), lhsT=aT[:, i // 4, i % 4, s * 128:(s + 1) * 128],
                                                 rhs=wdn[:, i // 4, i % 4, half * 512:(half + 1) * 512], start=(i == 0), stop=(i == 7))
                            return r
                        P.add("pe", mmso, reads=["aT0", "aT1", "wdn0", "wdn1"], writes=["ps%d" % bd, "ps%d" % (bd + 1)])
                        post_norm_res(ps[:, bd:bd + 2, :].rearrange("p b n -> p (b n)"), ["ps%d" % bd, "ps%d" % (bd + 1)], Gt,
                                      xres[:, s, :], ["xres%d" % s], tmps[s % 2], junk, st, "E", par=s % 2)
                        dma("sp", x2[t0 + s * 128:t0 + (s + 1) * 128, :], xres[:, s, :], ["xres%d" % s], ["x2_d"])
                    if dbg:
                        for s in range(8):
                            dma("sp", dbg_sc[t0 + s * 128:t0 + (s + 1) * 128, :], xres[:, s, :], ["xres%d" % s], ["dbg_sc"])
                    load_mod(1, 1, 0, At, Bt, None)
                    for s in range(8):
                        n = tt * 8 + s
                        hb_ = n % 2
                        norm_mod(xres[:, s, :], ["xres%d" % s], At, Bt, hn[:, hb_, :], tmp, junk, st, "E%d" % hb_, hn32=hn32[:])

                        def tr32(e):
                            for k in range(8):
                                r = e.transpose(out=ps[:, 4 + k // 4, (k % 4) * 128:(k % 4 + 1) * 128], in_=hn32[:, k * 128:(k + 1) * 128],
                                                identity=identf[:])
                            return r
                        P.add("pe", tr32, reads=["E%dhn32" % hb_, "ident"], writes=["ps4", "ps5"])
                        P.add("act", lambda e: e.activation(out=h32T[:].rearrange("p k t -> p (k t)"),
                                                            in_=ps[:, 4:6, :].rearrange("p b n -> p (b n)"), func=AF.Copy),
                              reads=["ps4", "ps5"], writes=["h32T"])

                        def mmr(e):
                            for k in range(8):
                                r = e.matmul(ps[:, 6, 0:NE], lhsT=h32T[:, k, :], rhs=rt[:, k, :], start=(k == 0), stop=(k == 7))
                            return r
                        P.add("pe", mmr, reads=["h32T", "rt"], writes=["ps6"])
                        l0 = lg[:, 0, :]
                        m1 = lg[:, 1, :]
                        l2 = lg[:, 2, :]
                        m2 = lg[:, 3, :]
                        m12 = lg[:, 4, :]
                        rk = lg[:, 5, :]
                        seq("dve", [
                            lambda e: e.tensor_copy(out=l0, in_=ps[:, 6, 0:NE]),
                            lambda e: e.tensor_reduce(out=sm[:, 0:1], in_=l0, axis=XA, op=ALU.max),
                            lambda e: e.tensor_scalar(out=m1, in0=l0, scalar1=sm[:, 0:1], scalar2=None, op0=ALU.is_equal),
                            lambda e: e.scalar_tensor_tensor(out=l2, in0=m1, scalar=-1e30, in1=l0, op0=ALU.mult, op1=ALU.add),
                            lambda e: e.tensor_reduce(out=sm[:, 1:2], in_=l2, axis=XA, op=ALU.max),
                            lambda e: e.tensor_scalar(out=m2, in0=l2, scalar1=sm[:, 1:2], scalar2=None, op0=ALU.is_equal),
                            lambda e: e.tensor_tensor(out=sm[:, 2:3], in0=sm[:, 1:2], in1=sm[:, 0:1], op=ALU.subtract),
                            lambda e: e.tensor_tensor(out=m12, in0=m1, in1=m2, op=ALU.add),
                        ], ["ps6"], ["lg"])
                        P.add("act", lambda e: e.activation(out=sm[:, 3:4], in_=sm[:, 2:3], func=AF.Exp), reads=["lg"], writes=["sm3"])
                        seq("dve", [
                            lambda e: e.tensor_scalar(out=sm[:, 4:5], in0=sm[:, 3:4], scalar1=1.0, scalar2=None, op0=ALU.add),
                            lambda e, n=n: e.reciprocal(out=gts[:, n, 0:1], in_=sm[:, 4:5]),
                            lambda e, n=n: e.tensor_tensor(out=gts[:, n, 1:2], in0=sm[:, 3:4], in1=gts[:, n, 0:1], op=ALU.mult),
                        ], ["sm3", "lg"], ["gts", "lg"])
                        def mmrank(e):
                            e.matmul(ps[:, 6, 64:64 + NE], lhsT=Lex[:], rhs=m12, start=True, stop=True)
                            return e.matmul(ps[:, 6, 128:128 + NE], lhsT=ones[:], rhs=m12, start=True, stop=True)
                        P.add("pe", mmrank, reads=["lg", "tri"], writes=["ps6"])
                        seq("dve", [
                            lambda e: e.tensor_tensor(out=rk, in0=ps[:, 6, 64:64 + NE], in1=carry[:], op=ALU.add),
                            lambda e: e.tensor_tensor(out=rk, in0=rk, in1=ebase[:], op=ALU.add),
                            lambda e: e.tensor_tensor(out=carry[:], in0=ps[:, 6, 128:128 + NE], in1=carry[:], op=ALU.add),
                            lambda e: e.tensor_tensor(out=l0, in0=rk, in1=m1, op=ALU.mult),
                            lambda e: e.tensor_reduce(out=sm[:, 6:7], in_=l0, axis=XA, op=ALU.add),
                            lambda e: e.tensor_tensor(out=l2, in0=rk, in1=m2, op=ALU.mult),
                            lambda e: e.tensor_reduce(out=sm[:, 7:8], in_=l2, axis=XA, op=ALU.add),
                            lambda e, n=n: e.tensor_copy(out=idx[0][n][:], in_=sm[:, 6:7]),
                            lambda e, n=n: e.tensor_copy(out=idx[1][n][:], in_=sm[:, 7:8]),
                        ], ["ps6", "carry", "ebase", "lg"], ["lg", "carry", "idx%d" % n])
                        for k in range(2):
                            P.add("pool", lambda e, k=k, n=n, hb_=hb_: e.indirect_dma_start(
                                out=hs[:, :], out_offset=bass.IndirectOffsetOnAxis(ap=idx[k][n][:, :], axis=0),
                                in_=hn[:, hb_, :].bitcast(F32), in_offset=None),
                                reads=["E%dhn" % hb_, "idx%d" % n], writes=["hs_d"], dma=True)
                ctf = sb("ctfE", [1, NE, NI], F32)
                cti = sb("ctiE", [1, NE, NI], I32)
                for i in range(NI):
                    P.add("dve", lambda e, i=i: e.tensor_scalar(out=ctf[:, :, i], in0=carry[0:1, :], scalar1=float(512 * i), scalar2=None,
                                                                op0=ALU.is_gt), reads=["carry"], writes=["ctf"])
                P.add("dve", lambda e: e.tensor_copy(out=cti[:], in_=ctf[:]), reads=["ctf"], writes=["cti"])
                dma("sp", ctab[:, :], cti[:].rearrange("o e i -> o (e i)"), ["cti"], ["ctab_d"])
                P.flush()

            if e_stop < 2:
                return
            with ExitStack() as es:
                def sb(name, shape, dt):
                    return es.enter_context(nc.sbuf_tensor(name, shape, dt))
                hsb = sb("hsbE2", [128, 2, 4, D], BF16)
                hT2 = sb("hTE2", [128, 2, 8, 512], BF16)
                aT = sb("aTE2", [128, 2, 4, 512], BF16)
                wgu = sb("wguE2", [128, 2, 8, 2, 512], BF16)
                wdn = sb("wdnE2", [128, 2, 4, D], BF16)
                yacc = sb("yaccE2", [128, 4, D], F32)
                stmp = sb("stmpE2", [128, 2, 512], F32)
                cnt = dict(blk=0, gu=0, dn=0, item=0)
                pend2 = [None]

                def wload(ex, bi):
                    wv = moe_w_gu[ex].rearrange("(k p) n -> p k n", p=128)
                    fc0 = bi * 4
                    b = cnt["blk"] % 2
                    cnt["blk"] += 1
                    dma("pool", wgu[:, b, :, 0, :], wv[:, :, fc0 * 128:(fc0 + 4) * 128], [], ["wgug%d" % b])
                    dma("pool", wgu[:, b, :, 1, :], wv[:, :, DFE + fc0 * 128:DFE + (fc0 + 4) * 128], [], ["wguu%d" % b])
                    dma("pool", wdn[:, b, :, :], moe_w_down[ex, fc0 * 128:(fc0 + 4) * 128, :].rearrange("(c p) d -> p c d", p=128),
                        [], ["wdn%d" % b])
                    return b

                def pf_rows(ex, it):
                    pb = cnt["item"] % 2
                    cnt["item"] += 1
                    r0 = ex * T + it * 512
                    dma("sp", hsb[:, pb].bitcast(F32), hs[r0:r0 + 512, :].rearrange("(s p) d -> p s d", p=128), ["hs_d"], ["hsb%d" % pb])
                    return pb

                def prefetch(ex, it, pb):
                    for s in range(4):
                        transposes_list([hsb[:, pb, s, k * 128:(k + 1) * 128] for k in range(8)], 7,
                                        hT2[:, pb, :, s * 128:(s + 1) * 128], ["hsb%d" % pb], ["hT%d" % pb])
                    b0 = wload(ex, 0)
                    return pb, b0

                for ex in range(NE):
                    nxt = prefetch(ex, 0, pf_rows(ex, 0))
                    for it in range(NI):
                        ci = ex * NI + it
                        P.begin_cond(ctab[0:1, ci:ci + 1], ["ctab_d"])
                        r0 = ex * T + it * 512
                        pb, b0 = nxt
                        if it + 1 < NI:
                            pbn = pf_rows(ex, it + 1)
                        hT = hT2[:, pb]
                        hTr = "hT%d" % pb
                        for bi in range(7):
                            b = b0 if bi == 0 else wload(ex, bi)
                            for j in range(4):
                                q = cnt["gu"] % 2
                                cnt["gu"] += 1
                                bg = q * 2
                                bu = q * 2 + 1

                                def mmgu(e, b=b, j=j, bg=bg, bu=bu, hT=hT):
                                    for k in range(8):
                                        e.matmul(psb(bg), lhsT=wgu[:, b, k, 0, j * 128:(j + 1) * 128], rhs=hT[:, k, :],
                                                 start=(k == 0), stop=(k == 7))
                                    for k in range(8):
                                        r = e.matmul(psb(bu), lhsT=wgu[:, b, k, 1, j * 128:(j + 1) * 128], rhs=hT[:, k, :],
                                                     start=(k == 0), stop=(k == 7))
                                    return r
                                P.add("pe", mmgu, reads=["wgug%d" % b, "wguu%d" % b, hTr], writes=["ps%d" % bg, "ps%d" % bu])
                                P.add("act", lambda e, q=q, bg=bg: e.activation(out=stmp[:, q, :], in_=psb(bg), func=AF.Silu),
                                      reads=["ps%d" % bg], writes=["stmp%d" % q])
                                P.add("dve", lambda e, q=q, bu=bu, b=b, j=j: e.tensor_tensor(
                                    out=aT[:, b, j, :], in0=psb(bu), in1=stmp[:, q, :], op=ALU.mult),
                                    reads=["ps%d" % bu, "stmp%d" % q], writes=["aT%d" % b])
                            def down(b=b, bi=bi):
                                for s in range(4):
                                    q = cnt["dn"] % 2
                                    cnt["dn"] += 1
                                    bd = 4 if q == 0 else 6

                                    def mmdn(e, b=b, s=s, bd=bd):
                                        for half in range(2):
                                            for j in range(4):
                                                r = e.matmul(psb(bdm(bd, half)), lhsT=aT[:, b, j, s * 128:(s + 1) * 128],
                                                             rhs=wdn[:, b, j, half * 512:(half + 1) * 512], start=(j == 0), stop=(j == 3))
                                        return r
                                    P.add("pe", mmdn, reads=["aT%d" % b, "wdn%d" % b], writes=["ps%d" % bdm(bd, 0), "ps%d" % bdm(bd, 1)])
                                    for half in range(2):
                                        hs_ = slice(half * 512, (half + 1) * 512)
                                        if bi == 0:
                                            P.add("dve", lambda e, s=s, half=half, hs_=hs_, bd=bd: e.tensor_copy(out=yacc[:, s, hs_], in_=psb(bdm(bd, half))),
                                                  reads=["ps%d" % bdm(bd, half)], writes=["yacc%d" % s])
                                        else:
                                            P.add("dve", lambda e, s=s, half=half, hs_=hs_, bd=bd: e.tensor_tensor(
                                                out=yacc[:, s, hs_], in0=psb(bdm(bd, half)), in1=yacc[:, s, hs_], op=ALU.add),
                                                reads=["ps%d" % bdm(bd, half), "yacc%d" % s], writes=["yacc%d" % s])
                            if pend2[0] is not None:
                                pend2[0]()
                            pend2[0] = down
                        if it + 1 < NI:
                            nxt = prefetch(ex, it + 1, pbn)
                        pend2[0]()
                        pend2[0] = None
                        for s in range(4):
                            dma("sp", ys[r0 + s * 128:r0 + (s + 1) * 128, :], yacc[:, s, :], ["yacc%d" % s], ["ys_d"])
                        P.end_cond()
                P.flush()

            if e_stop < 3:
                return
            with ExitStack() as es:
                def sb(name, shape, dt):
                    return es.enter_context(nc.sbuf_tensor(name, shape, dt))
                Gt = sb("GtE3", [128, D], F32)
                NB3 = 4
                xr = sb("xrE3", [128, NB3, D], F32)
                y1 = sb("y1E3", [128, NB3, D], F32)
                y2 = sb("y2E3", [128, NB3, D], F32)
                tmp3 = [sb("tmpE3a", [128, D], F32), sb("tmpE3b", [128, D], F32)]
                junk = sb("junkE3", [128, D], BF16)
                st = sb("stE3", [128, 16], F32)
                load_mod(1, 1, 0, None, None, Gt)

                def loads(n):
                    b = n % NB3
                    rows = slice(n * 128, (n + 1) * 128)
                    dma("sp", xr[:, b, :], x2[rows, :], ["x2_d"], ["xr%d" % b])
                    P.add("pool", lambda e: e.indirect_dma_start(
                        out=y1[:, b, :], out_offset=None, in_=ys[:, :],
                        in_offset=bass.IndirectOffsetOnAxis(ap=idx[0][n][:, :], axis=0)),
                        reads=["ys_d"], writes=["y1%d" % b], dma=True)
                    P.add("pool", lambda e: e.indirect_dma_start(
                        out=y2[:, b, :], out_offset=None, in_=ys[:, :],
                        in_offset=bass.IndirectOffsetOnAxis(ap=idx[1][n][:, :], axis=0)),
                        reads=["ys_d"], writes=["y2%d" % b], dma=True)
                for n in range(min(NB3 - 1, NS)):
                    loads(n)
                for n in range(NS):
                    if n + NB3 - 1 < NS:
                        loads(n + NB3 - 1)
                    b = n % NB3
                    par = n % 2
                    c0 = 8 * par
                    rows = slice(n * 128, (n + 1) * 128)
                    P.add("dve", lambda e, n=n, b=b: e.tensor_scalar(out=y1[:, b, :], in0=y1[:, b, :], scalar1=gts[:, n, 0:1], scalar2=None,
                                                                     op0=ALU.mult), reads=["y1%d" % b], writes=["y1%d" % b])
                    P.add("dve", lambda e, n=n, b=b: e.scalar_tensor_tensor(out=y1[:, b, :], in0=y2[:, b, :], scalar=gts[:, n, 1:2], in1=y1[:, b, :],
                                                                            op0=ALU.mult, op1=ALU.add),
                          reads=["y1%d" % b, "y2%d" % b], writes=["y1%d" % b])
                    tg = "E3p%d" % par
                    rstd_ops(y1[:, b, :], junk[:], st[:, c0 + 2:c0 + 3], st[:, c0 + 3:c0 + 4], D, ["y1%d" % b], tg)
                    P.add("dve", lambda e, b=b, c0=c0, par=par: e.scalar_tensor_tensor(out=tmp3[par][:], in0=y1[:, b, :], scalar=st[:, c0 + 3:c0 + 4],
                                                                                     in1=Gt[:], op0=ALU.mult, op1=ALU.mult),
                          reads=["y1%d" % b, tg + "rs", "modtiles"], writes=[tg + "tmp"])
                    P.add("dve", lambda e, b=b, par=par: e.tensor_tensor(out=xr[:, b, :], in0=xr[:, b, :], in1=tmp3[par][:], op=ALU.add),
                          reads=[tg + "tmp", "xr%d" % b], writes=["xr%d" % b])
                    dma("sp", out[rows, :], xr[:, b, :], ["xr%d" % b], ["out_d"])
                P.flush()

    if "A" in phases:
        phase_A()
    if "B" in phases:
        phase_B()
    if "C" in phases:
        phase_C()
    if "D" in phases:
        phase_D()
    if "E" in phases:
        phase_E()
    scratch = dict(modv=modv, x_tok=x_tok, B_tok=B_tok, BT=BT, CT=CT, sz=sz, dts=dts, hb=hb, x1=x1, hf0=hf0)
    return nc, scratch


_CACHE = {}


def _prep_inputs(inputs, b, T):
    f = lambda a: np.ascontiguousarray(a, dtype=np.float32)
    m = {
        "x": f(inputs["x"][b, :T]),
        "c": f(inputs["c"][b]),
        "ctx": f(inputs["ctx"][b]),
        "c_ctx": f(inputs["c_ctx"]),
        "ada_w": f(inputs["ada_w"]),
        "ada_b": f(inputs["ada_b"]),
        "norm_pre": f(np.reshape(inputs["norm_pre"], (2, 2 * D))),
        "norm_post": f(np.reshape(inputs["norm_post"], (2, 2 * D))),
        "ssd_w_in": f(inputs["ssd_w_in"][0]),
        "ssd_conv_w": f(inputs["ssd_conv_w"][0]),
        "ssd_conv_b": f(inputs["ssd_conv_b"][0]),
        "ssd_a_log": f(np.reshape(inputs["ssd_a_log"][0], (1, 64))),
        "ssd_dt_bias": f(np.reshape(inputs["ssd_dt_bias"][0], (1, 64))),
        "ssd_d": f(np.reshape(inputs["ssd_d"][0], (1, 32))),
        "ssd_norm": f(np.reshape(inputs["ssd_norm"][0], (1, DI))),
        "ssd_w_out": f(inputs["ssd_w_out"][0]),
        "sc_w_in": f(inputs["sc_w_in"][0]),
        "sc_conv_w": f(inputs["sc_conv_w"][0]),
        "sc_w_out": f(inputs["sc_w_out"][0]),
        "ffn_w_gu": f(inputs["ffn_w_gu"][0]),
        "ffn_w_down": f(inputs["ffn_w_down"][0]),
        "moe_router": f(inputs["moe_router"][0]),
        "moe_w_gu": f(inputs["moe_w_gu"][0]),
        "moe_w_down": f(inputs["moe_w_down"][0]),
    }
    return m


def kernel(**inputs):
    x = np.asarray(inputs["x"])
    Bn, T, _ = x.shape
    if T not in _CACHE:
        _CACHE[T] = build(T)[0]
    nc = _CACHE[T]
    shared = _prep_inputs(inputs, 0, T)
    in_maps = []
    for b in range(Bn):
        m = dict(shared)
        m["x"] = np.ascontiguousarray(x[b], dtype=np.float32)
        m["c"] = np.ascontiguousarray(np.asarray(inputs["c"])[b], dtype=np.float32)
        m["ctx"] = np.ascontiguousarray(np.asarray(inputs["ctx"])[b], dtype=np.float32)
        in_maps.append(m)
    res = run_bass_kernel_spmd(nc, in_maps, core_ids=list(range(Bn)))
    return np.stack([np.asarray(r["out"], dtype=np.float32) for r in res.results], axis=0)
```

```python
import numpy as np
import concourse.bass as bass
import concourse.mybir as mybir
from concourse.bass_utils import run_bass_kernel_spmd
from contextlib import ExitStack

F32 = mybir.dt.float32
BF16 = mybir.dt.bfloat16
AF = mybir.ActivationFunctionType
ALU = mybir.AluOpType

ENGS = ("pe", "act", "dve", "pool", "sp")
SAME_ENGINE_SYNC = {"pe": False, "act": True, "dve": True, "pool": True, "sp": False}

D = 1024
KD = 8
TC = 256
DI = 2048
NH = 32
NG = 8
HP = 64
DIN = 6208
DFF = 2816
NE = 8
DFE = 3584
EPS = 1e-6
NEG = -30000.0
NSIDE = 2


class Prog:
    def __init__(self, nc, n_dma_sems=48):
        self.nc = nc
        self.ops = []
        self.eng_sem = {e: nc.alloc_semaphore("prog_" + e) for e in ("pe", "act", "dve", "pool")}
        self.eng_cnt = {e: 0 for e in ("pe", "act", "dve", "pool")}
        self.dma_sems = [nc.alloc_semaphore("dma%d" % i) for i in range(n_dma_sems)]
        self.dma_val = [0] * n_dma_sems
        self.dma_rr = 0
        self.dma_rr_sw = 0
        self.last_w = {}
        self.readers = {}
        self.waited = {e: {} for e in ENGS}
        self.region = None
        self.regs = {}

    def begin_cond(self, cond_ap, dep_reads):
        self.region = dict(cond=cond_ap, deps=list(dep_reads), pre={}, start_cnt=dict(self.eng_cnt))
        self._snap = {e: dict(w) for e, w in self.waited.items()}

    def end_cond(self):
        self.region = None
        self.waited = self._snap

    def add(self, eng, fn, reads=(), writes=(), dma=False, n_dma=1):
        op = dict(eng=eng, fn=fn, dma=dma, waits=[], n_dma=n_dma, region=self.region)
        if self.region is not None and eng not in self.region["pre"]:
            pre = dict(eng=eng, waits=[])
            for r in self.region["deps"]:
                w = self.last_w.get(r)
                if w is not None:
                    self._want(pre, *w["sig"])
            self.region["pre"][eng] = pre["waits"]
        deps = []
        for r in reads:
            w = self.last_w.get(r)
            if w is not None:
                deps.append(w)
        for r in writes:
            w = self.last_w.get(r)
            if w is not None:
                deps.append(w)
            deps.extend(self.readers.get(r, ()))
        for r in reads:
            self.readers.setdefault(r, []).append(op)
        for r in writes:
            self.last_w[r] = op
            self.readers[r] = []
        if dma:
            n_sw = 16
            if eng == "pool":
                si = self.dma_rr_sw
                self.dma_rr_sw = (self.dma_rr_sw + 1) % n_sw
            else:
                si = n_sw + self.dma_rr
                self.dma_rr = (self.dma_rr + 1) % (len(self.dma_sems) - n_sw)
            prev = self.dma_val[si]
            op["dma_prev"] = (si, prev)
            if prev > 0:
                self._want(op, ("dma", si), prev)
            self.dma_val[si] = prev + 16 * n_dma
            op["sig"] = (("dma", si), self.dma_val[si])
        else:
            op["cnt_before"] = self.eng_cnt[eng]
            self.eng_cnt[eng] += 1
            op["sig"] = (("eng", eng), self.eng_cnt[eng])
        for d in deps:
            if d is op:
                continue
            key, val = d["sig"]
            if key[0] == "eng" and key[1] == eng and not SAME_ENGINE_SYNC[eng]:
                continue
            self._want(op, key, val)
        self.ops.append(op)
        return op

    def _want(self, op, key, val):
        w = self.waited[op["eng"]]
        if w.get(key, 0) >= val:
            return
        w[key] = val
        op["waits"].append((key, val))

    def _sem(self, key):
        return self.eng_sem[key[1]] if key[0] == "eng" else self.dma_sems[key[1]]

    def flush(self):
        nc = self.nc
        ops = self.ops
        self.ops = []
        by_eng = {e: [o for o in ops if o["eng"] == e] for e in ENGS}
        finals = [(("eng", e), self.eng_cnt[e]) for e in self.eng_cnt if self.eng_cnt[e] > 0]
        finals += [(("dma", i), v) for i, v in enumerate(self.dma_val) if v > 0]

        def emit_op(eng, o):
            for key, val in o["waits"]:
                eng.wait_ge(self._sem(key), val)
            r = o["fn"](eng)
            key, val = o["sig"]
            if o["dma"]:
                rs = r if isinstance(r, (list, tuple)) else [r]
                assert len(rs) == o["n_dma"], (len(rs), o["n_dma"])
                for i in rs:
                    i.then_inc(self._sem(key), 16)
            else:
                assert r is not None
                r.then_inc(self._sem(key), 1)

        def emit(eng_name, eng):
            lst = by_eng[eng_name]
            i = 0
            while i < len(lst):
                o = lst[i]
                R = o["region"]
                if R is None:
                    emit_op(eng, o)
                    i += 1
                    continue
                j = i
                while j < len(lst) and lst[j]["region"] is R:
                    j += 1
                group = lst[i:j]
                i = j
                for key, val in R["pre"][eng_name]:
                    eng.wait_ge(self._sem(key), val)
                if eng_name not in self.regs:
                    self.regs[eng_name] = eng.alloc_register("cond_" + eng_name)
                reg = self.regs[eng_name]
                eng.reg_load(reg, R["cond"])
                with eng.If_ne(reg, 0):
                    for o2 in group:
                        emit_op(eng, o2)
                with eng.Else():
                    ncomp = [o2 for o2 in group if not o2["dma"]]
                    if ncomp:
                        c0 = ncomp[0]["cnt_before"]
                        if c0 > 0:
                            eng.wait_ge(self.eng_sem[eng_name], c0)
                        eng.sem_inc(self.eng_sem[eng_name], len(ncomp))
                    for o2 in group:
                        if o2["dma"]:
                            si, prev = o2["dma_prev"]
                            if prev > 0:
                                eng.wait_ge(self.dma_sems[si], prev)
                            eng.sem_inc(self.dma_sems[si], 16 * o2["n_dma"])
            w = self.waited[eng_name]
            for key, val in finals:
                if key == ("eng", eng_name):
                    continue
                if w.get(key, 0) < val:
                    w[key] = val
                    eng.wait_ge(self._sem(key), val)

        with nc.Block() as block:
            @block.tensor
            def _(e):
                emit("pe", e)

            @block.scalar
            def _(e):
                emit("act", e)

            @block.vector
            def _(e):
                emit("dve", e)

            @block.gpsimd
            def _(e):
                emit("pool", e)

            @block.sync
            def _(e):
                emit("sp", e)
        self.last_w = {}
        self.readers = {}


def bview(ap, h, q):
    return ap.rearrange("p (h q) -> p h q", h=h)


def build(T=4096, phases="ABCDE", dbg=False, e_stop=3, d_stop=99):
    NCH = T // 128
    TT = T + TC
    nc = bass.Bass("TRN2", target_bir_lowering=False)
    dt_ = nc.dram_tensor

    def inp(name, shape):
        return dt_(name, list(shape), F32, kind="ExternalInput").ap()

    x_in = inp("x", [T, D])
    c_in = inp("c", [D])
    ctx_in = inp("ctx", [TC, D])
    cctx_in = inp("c_ctx", [D])
    ada_w = inp("ada_w", [2, D, 6 * D])
    ada_b = inp("ada_b", [2, 6 * D])
    norm_pre = inp("norm_pre", [2, 2 * D])
    norm_post = inp("norm_post", [2, 2 * D])
    ssd_w_in = inp("ssd_w_in", [D, DIN])
    ssd_conv_w = inp("ssd_conv_w", [5, 4096])
    ssd_conv_b = inp("ssd_conv_b", [4096])
    ssd_a_log = inp("ssd_a_log", [1, 64])
    ssd_dt_bias = inp("ssd_dt_bias", [1, 64])
    ssd_d = inp("ssd_d", [1, 32])
    ssd_norm = inp("ssd_norm", [1, DI])
    ssd_w_out = inp("ssd_w_out", [DI, D])
    sc_w_in = inp("sc_w_in", [D, 3 * D])
    sc_conv_w = inp("sc_conv_w", [3, D])
    sc_w_out = inp("sc_w_out", [D, D])
    ffn_w_gu = inp("ffn_w_gu", [D, 2 * DFF])
    ffn_w_down = inp("ffn_w_down", [DFF, D])
    moe_router = inp("moe_router", [D, NE])
    moe_w_gu = inp("moe_w_gu", [NE, D, 2 * DFE])
    moe_w_down = inp("moe_w_down", [NE, DFE, D])
    out = dt_("out", [T, D], F32, kind="ExternalOutput").ap()

    sk = "ExternalOutput" if dbg else "Internal"
    modv = dt_("modv", [2, 2, 3, 2, D], F32, kind=sk).ap()
    x_tok = dt_("x_tok", [TT, DI], BF16, kind=sk).ap()
    B_tok = dt_("B_tok", [TT, 1024], BF16, kind=sk).ap()
    BT = dt_("BT", [1024, T], BF16, kind=sk).ap()
    CT = dt_("CT", [1024, T], BF16, kind=sk).ap()
    sz = dt_("sz", [T, DI], BF16, kind=sk).ap()
    dts = dt_("dts", [TT, 64], F32, kind=sk).ap()
    hb = dt_("hb", [NCH, 128, DI], BF16, kind=sk).ap()
    x1 = dt_("x1", [T, D], F32, kind=sk).ap()
    hf0 = dt_("hf0", [128, DI], F32, kind=sk).ap()
    if dbg:
        dbg_ffn = dt_("dbg_ffn", [T, D], F32, kind=sk).ap()
        dbg_sc = dt_("dbg_sc", [T, D], F32, kind=sk).ap()
        dbg_wg = dt_("dbg_wg", [T, NE], F32, kind=sk).ap()

    P = Prog(nc)
    ps = nc.alloc_psum_tensor("ps", [128, 8, 512], F32)
    identb = nc.alloc_sbuf_tensor("identb", [128, 128], BF16)
    identf = nc.alloc_sbuf_tensor("identf", [128, 128], F32)

    def psb(b):
        return ps[:, b, :]

    def psb16(b):
        return ps[:, b, :].bitcast(BF16)

    def dma(eng, out_ap, in_ap, reads, writes):
        P.add(eng, lambda e: e.dma_start(out=out_ap, in_=in_ap), reads=reads, writes=writes, dma=True)

    def dma_slow(eng, out_ap, in_ap, reads, writes):
        P.add(eng, lambda e: e.dma_start(out=out_ap, in_=in_ap, allow_slow_non_contiguous=True),
              reads=reads, writes=writes, dma=True)

    def seq(eng, fns, reads, writes):
        for f in fns:
            P.add(eng, f, reads=reads, writes=writes)

    seq("pool", [
        lambda e: e.memset(identf[:], 0.0),
        lambda e: e.affine_select(out=identf[:], in_=identf[:], pattern=[[-1, 128]], compare_op=ALU.not_equal,
                                  fill=1.0, base=0, channel_multiplier=1),
        lambda e: e.memset(identb[:], 0.0),
        lambda e: e.affine_select(out=identb[:], in_=identb[:], pattern=[[-1, 128]], compare_op=ALU.not_equal,
                                  fill=1.0, base=0, channel_multiplier=1),
    ], [], ["ident"])
    P.flush()

    def rstd_ops(src_ap, junk_ap, ss_ap, rs_ap, n, rd, tag):
        P.add("act", lambda e: e.activation(out=junk_ap, in_=src_ap, func=AF.Square, accum_out=ss_ap),
              reads=rd, writes=[tag + "junk", tag + "ss"])
        P.add("act", lambda e: e.activation(out=rs_ap, in_=ss_ap, func=AF.Sqrt, bias=EPS, scale=1.0 / n),
              reads=[tag + "ss"], writes=[tag + "rs"])
        P.add("dve", lambda e: e.reciprocal(out=rs_ap, in_=rs_ap), reads=[tag + "rs"], writes=[tag + "rs"])

    def norm_mod(x_ap, x_res, A_t, B_t, hn_ap, tmp, junk, st, tag, hn32=None, par=0):
        tag = tag + ("p%d" % par if par else "")
        c0 = 8 * par
        rstd_ops(x_ap, junk[:], st[:, c0:c0 + 1], st[:, c0 + 1:c0 + 2], D, x_res, tag)
        P.add("dve", lambda e: e.scalar_tensor_tensor(out=tmp[:], in0=x_ap, scalar=st[:, c0 + 1:c0 + 2], in1=A_t[:],
                                                      op0=ALU.mult, op1=ALU.mult),
              reads=x_res + [tag + "rs", "modtiles"], writes=[tag + "tmp"])
        if hn32 is not None:
            P.add("pool", lambda e: e.tensor_tensor(out=hn32, in0=tmp[:], in1=B_t[:], op=ALU.add),
                  reads=[tag + "tmp", "modtiles"], writes=[tag + "hn32"])
            P.add("act", lambda e: e.activation(out=hn_ap, in_=hn32, func=AF.Copy),
                  reads=[tag + "hn32"], writes=[tag + "hn"])
        else:
            P.add("pool", lambda e: e.tensor_tensor(out=hn_ap, in0=tmp[:], in1=B_t[:], op=ALU.add),
                  reads=[tag + "tmp", "modtiles"], writes=[tag + "hn"])
        return tag + "hn"

    def transposes_to(hn_ap, nk, bank, dst_ap, rd, wr, eng="act"):
        nb = (nk + 7) // 8
        banks = [bank + i for i in range(nb)]

        def tr(e):
            for k in range(nk):
                r = e.transpose(out=psb16(bank + k // 8)[:, (k % 8) * 128:(k % 8 + 1) * 128],
                                in_=hn_ap[:, k * 128:(k + 1) * 128], identity=identb[:])
            return r
        P.add("pe", tr, reads=rd + ["ident"], writes=["ps%d" % b for b in banks])
        for i, b in enumerate(banks):
            k0 = i * 8
            k1 = min(nk, k0 + 8)
            src = psb16(b)[:, 0:(k1 - k0) * 128].rearrange("p (k t) -> p k t", t=128)
            dsl = dst_ap[:, k0:k1, :]
            if eng == "act":
                P.add("act", lambda e, s=src, d_=dsl: e.activation(out=d_, in_=s, func=AF.Copy),
                      reads=["ps%d" % b], writes=wr)
            else:
                P.add("dve", lambda e, s=src, d_=dsl: e.tensor_copy(out=d_, in_=s),
                      reads=["ps%d" % b], writes=wr)

    def load_mod(i, s, r, A_t, B_t, G_t):
        for q, t in ((0, A_t), (1, B_t), (2, G_t)):
            if t is None:
                continue
            dma("sp", t[:], modv[i, s, q, r:r + 1, :].partition_broadcast(128), ["modv"], ["modtiles"])

    def post_norm_res(y_ap, y_res, G_t, xres_ap, xres_res, tmp, junk, st, tag, par=0):
        tag = tag + ("q%d" % par if par else "")
        c0 = 8 * par
        rstd_ops(y_ap, junk[:], st[:, c0 + 2:c0 + 3], st[:, c0 + 3:c0 + 4], D, y_res, tag + "p")
        P.add("dve", lambda e: e.scalar_tensor_tensor(out=tmp[:], in0=y_ap, scalar=st[:, c0 + 3:c0 + 4], in1=G_t[:],
                                                      op0=ALU.mult, op1=ALU.mult),
              reads=y_res + [tag + "prs", "modtiles"], writes=[tag + "tmp"])
        P.add("pool", lambda e: e.tensor_tensor(out=xres_ap, in0=xres_ap, in1=tmp[:], op=ALU.add),
              reads=[tag + "tmp"] + xres_res, writes=xres_res)

    def transposes_list(in_aps, bank, dst_ap, rd, wr, eng="act"):
        nk = len(in_aps)
        nb = (nk + 7) // 8
        banks = [bank + i for i in range(nb)]

        def tr(e):
            for k in range(nk):
                r = e.transpose(out=psb16(bank + k // 8)[:, (k % 8) * 128:(k % 8 + 1) * 128],
                                in_=in_aps[k], identity=identb[:])
            return r
        P.add("pe", tr, reads=rd + ["ident"], writes=["ps%d" % b for b in banks])
        for i, b in enumerate(banks):
            k0 = i * 8
            k1 = min(nk, k0 + 8)
            src = psb16(b)[:, 0:(k1 - k0) * 128].rearrange("p (k t) -> p k t", t=128)
            dsl = dst_ap[:, k0:k1, :]
            if eng == "act":
                P.add("act", lambda e, s=src, d_=dsl: e.activation(out=d_, in_=s, func=AF.Copy),
                      reads=["ps%d" % b], writes=wr)
            else:
                P.add("dve", lambda e, s=src, d_=dsl: e.tensor_copy(out=d_, in_=s),
                      reads=["ps%d" % b], writes=wr)

    tri_id = [0]

    def tri_consts(es):
        tri_id[0] += 1
        Lex = es.enter_context(nc.sbuf_tensor("Lex%d" % tri_id[0], [128, 128], F32))
        Uex = es.enter_context(nc.sbuf_tensor("Uex%d" % tri_id[0], [128, 128], F32))
        ones = es.enter_context(nc.sbuf_tensor("ones%d" % tri_id[0], [128, 128], F32))
        A_b = es.enter_context(nc.sbuf_tensor("A_b%d" % tri_id[0], [128, 64], F32))

        seq("pool", [
            lambda e: e.memset(ones[:], 1.0),
            lambda e: e.memset(Lex[:], 1.0),
            lambda e: e.affine_select(out=Lex[:], in_=Lex[:], pattern=[[1, 128]], compare_op=ALU.is_gt, fill=0.0,
                                      base=0, channel_multiplier=-1),
            lambda e: e.memset(Uex[:], 1.0),
            lambda e: e.affine_select(out=Uex[:], in_=Uex[:], pattern=[[-1, 128]], compare_op=ALU.is_gt, fill=0.0,
                                      base=0, channel_multiplier=1),
        ], [], ["tri"])
        dma("sp", A_b[:], ssd_a_log[0:1, :].partition_broadcast(128), [], ["A_b"])
        P.add("act", lambda e: e.activation(out=A_b[:], in_=A_b[:], func=AF.Exp), reads=["A_b"], writes=["A_b"])
        P.add("dve", lambda e: e.tensor_scalar(out=A_b[:], in0=A_b[:], scalar1=-1.0, scalar2=None, op0=ALU.mult),
              reads=["A_b"], writes=["A_b"])
        return Lex, Uex, ones, A_b

    def phase_A():
        with ExitStack() as es:
            def sb(name, shape, dt):
                return es.enter_context(nc.sbuf_tensor(name, shape, dt))
            cT = sb("cT", [128, 8, 2], F32)
            wa = sb("wa", [128, 2, 8, 512], F32)
            modraw = sb("modraw", [2, 6 * D], F32)
            adab = sb("adab", [2, 6 * D], F32)
            npre = sb("npre", [2, 2 * D], F32)
            npost = sb("npost", [2, 2 * D], F32)
            outv = sb("outv", [2, 6, D], F32)
            dma_slow("sp", cT[:, :, 0], c_in.rearrange("(k p) -> p k", p=128), [], ["cT"])
            dma_slow("sp", cT[:, :, 1], cctx_in.rearrange("(k p) -> p k", p=128), [], ["cT"])
            P.add("act", lambda e: e.activation(out=cT[:], in_=cT[:], func=AF.Silu), reads=["cT"], writes=["cT"])
            for i in range(2):
                dma("sp", adab[:], ada_b[i:i + 1, :].partition_broadcast(2), [], ["adab"])
                dma("sp", npre[:], norm_pre[i:i + 1, :].partition_broadcast(2), [], ["npre"])
                dma("sp", npost[:], norm_post[i:i + 1, :].partition_broadcast(2), [], ["npost"])
                wv = ada_w[i].rearrange("(k p) n -> p k n", p=128)
                for ct in range(12):
                    b = ct % 2
                    dma("sp", wa[:, b], wv[:, :, ct * 512:(ct + 1) * 512], [], ["wa%d" % b])

                    def mm(e, b=b):
                        for k in range(8):
                            r = e.matmul(ps[0:2, b, :], lhsT=cT[:, k, :], rhs=wa[:, b, k, :], start=(k == 0), stop=(k == 7))
                        return r
                    P.add("pe", mm, reads=["cT", "wa%d" % b], writes=["ps%d" % b])
                    P.add("dve", lambda e, b=b, ct=ct: e.tensor_tensor(out=modraw[:, ct * 512:(ct + 1) * 512], in0=ps[0:2, b, :],
                                                                       in1=adab[:, ct * 512:(ct + 1) * 512], op=ALU.add),
                          reads=["ps%d" % b, "adab"], writes=["modraw"])
                for s in range(2):
                    sh = modraw[:, (3 * s) * D:(3 * s + 1) * D]
                    sc = modraw[:, (3 * s + 1) * D:(3 * s + 2) * D]
                    gg = modraw[:, (3 * s + 2) * D:(3 * s + 3) * D]
                    P.add("dve", lambda e, s=s, sc=sc: e.scalar_tensor_tensor(out=outv[:, 3 * s + 0, :], in0=sc, scalar=1.0,
                                                                              in1=npre[:, s * D:(s + 1) * D], op0=ALU.add, op1=ALU.mult),
                          reads=["modraw", "npre"], writes=["outv"])
                    P.add("dve", lambda e, s=s, sh=sh: e.tensor_copy(out=outv[:, 3 * s + 1, :], in_=sh),
                          reads=["modraw"], writes=["outv"])
                    P.add("dve", lambda e, s=s, gg=gg: e.tensor_tensor(out=outv[:, 3 * s + 2, :], in0=gg,
                                                                       in1=npost[:, s * D:(s + 1) * D], op=ALU.mult),
                          reads=["modraw", "npost"], writes=["outv"])
                dma("sp", modv[i].rearrange("s q r d -> r (s q) d"), outv[:], ["outv"], ["modv"])
            P.flush()

    def phase_B():
        with ExitStack() as es:
            def sb(name, shape, dt):
                return es.enter_context(nc.sbuf_tensor(name, shape, dt))
            w_in = sb("w_in", [128, 8, DIN], BF16)
            modt = [sb("modB%d" % i, [128, D], F32) for i in range(4)]
            xin = sb("xinB", [128, 2, D], F32)
            tmpsB = [sb("tmpB", [128, D], F32), sb("tmpB2", [128, D], F32)]
            junk = sb("junkB", [128, D], BF16)
            st = sb("stB", [128, 16], F32)
            hn = sb("hnB", [128, 2, D], BF16)
            hT = sb("hTB", [128, 2, 8, 512], BF16)
            cacc = sb("cacc", [128, 2, 512], F32)
            cacc2 = sb("cacc2", [128, 512], F32)
            ucp = sb("ucp", [128, 512], F32)
            xbcT = sb("xbcT", [128, 16, 512], BF16)
            szo = sb("szo", [128, 2, DI], BF16)
            xto = sb("xto", [128, 2, DI], BF16)
            bto = sb("bto", [128, 2, 1024], BF16)
            dtt = sb("dtt", [128, 2, 64], F32)
            cw = sb("cw", [128, 32, 5], F32)
            cb = sb("cb", [128, 32], F32)
            dtb = sb("dtb", [128, 64], F32)
            for k in range(8):
                dma("pool", w_in[:, k, :], ssd_w_in[k * 128:(k + 1) * 128, :], [], ["w_in%d" % k])
            for t in range(5):
                dma_slow("sp", cw[:, :, t], ssd_conv_w[t].rearrange("(c p) -> p c", p=128), [], ["cw"])
            dma_slow("sp", cb[:], ssd_conv_b.rearrange("(c p) -> p c", p=128), [], ["cw"])
            dma("sp", dtb[:], ssd_dt_bias[0:1, :].partition_broadcast(128), [], ["dtb"])
            load_mod(0, 0, 0, modt[0], modt[1], None)
            load_mod(0, 0, 1, modt[2], modt[3], None)
            tiles = [(ctx_in, 0, 256, 256, True, T)]
            for j in range(T // 512):
                tiles.append((x_in, j * 512, 512, 64, False, j * 512))
            subc = [0]
            zc_cnt = [0]
            for ti, (src, r0, W, RW, is_ctx, srow) in enumerate(tiles):
                hb_ = ti % 2
                A_t, B_t = (modt[2], modt[3]) if is_ctx else (modt[0], modt[1])
                nsub = W // 128
                def nmB(s):
                    b = subc[0] % 2
                    subc[0] += 1
                    dma("sp", xin[:, b, :], src[r0 + s * 128:r0 + (s + 1) * 128, :], [], ["xin%d" % b])
                    return b, norm_mod(xin[:, b, :], ["xin%d" % b], A_t, B_t, hn[:, b, :], tmpsB[b], junk, st, "B", par=b)
                cur = nmB(0)
                for s in range(nsub):
                    nxt = nmB(s + 1) if s + 1 < nsub else None
                    b, hres = cur
                    transposes_list([hn[:, b, k * 128:(k + 1) * 128] for k in range(8)], 0,
                                    hT[:, hb_, :, s * 128:(s + 1) * 128], [hres], ["hT%d" % hb_])
                    cur = nxt
                for s in range(nsub):
                    b = subc[0] % 2
                    subc[0] += 1
                    rows = slice(srow + s * 128, srow + (s + 1) * 128)
                    if not is_ctx:
                        for zc in range(4):
                            bk = 1 + zc_cnt[0] % 2
                            zc_cnt[0] += 1

                            def mmz(e, bk=bk, s=s, zc=zc, hb_=hb_):
                                for k in range(8):
                                    r = e.matmul(psb(bk), lhsT=hT[:, hb_, k, s * 128:(s + 1) * 128],
                                                 rhs=w_in[:, k, zc * 512:(zc + 1) * 512], start=(k == 0), stop=(k == 7))
                                return r
                            P.add("pe", mmz, reads=["hT%d" % hb_] + ["w_in%d" % k_ for k_ in range(8)], writes=["ps%d" % bk])
                            P.add("act", lambda e, bk=bk, b=b, zc=zc: e.activation(out=szo[:, b, zc * 512:(zc + 1) * 512],
                                                                                   in_=psb(bk), func=AF.Silu),
                                  reads=["ps%d" % bk], writes=["szo%d" % b])
                        dma("act", sz[rows, :], szo[:, b, :], ["szo%d" % b], ["sz_d"])
                    bk = 1 + zc_cnt[0] % 2
                    zc_cnt[0] += 1

                    def mmdt(e, bk=bk, s=s, hb_=hb_):
                        for k in range(8):
                            r = e.matmul(psb(bk)[:, 0:64], lhsT=hT[:, hb_, k, s * 128:(s + 1) * 128],
                                         rhs=w_in[:, k, 6144:6208], start=(k == 0), stop=(k == 7))
                        return r
                    P.add("pe", mmdt, reads=["hT%d" % hb_] + ["w_in%d" % k_ for k_ in range(8)], writes=["ps%d" % bk])
                    P.add("dve", lambda e, bk=bk, b=b: e.tensor_tensor(out=dtt[:, b, :], in0=psb(bk)[:, 0:64], in1=dtb[:], op=ALU.add),
                          reads=["ps%d" % bk, "dtb"], writes=["dtt%d" % b])
                    P.add("act", lambda e, b=b: e.activation(out=dtt[:, b, :], in_=dtt[:, b, :], func=AF.Exp),
                          reads=["dtt%d" % b], writes=["dtt%d" % b])
                    P.add("act", lambda e, b=b: e.activation(out=dtt[:, b, :], in_=dtt[:, b, :], func=AF.Ln, bias=1.0),
                          reads=["dtt%d" % b], writes=["dtt%d" % b])
                    dma("act", dts[rows, :], dtt[:, b, :], ["dtt%d" % b], ["dts_d"])
                ncc = 24 if is_ctx else 32
                for cc in range(ncc):
                    bk = 3 + cc % 2
                    slot = cc % 16

                    def mmx(e, bk=bk, cc=cc, hb_=hb_, W=W):
                        for k in range(8):
                            r = e.matmul(psb(bk)[:, 0:W], lhsT=w_in[:, k, 2048 + cc * 128:2048 + (cc + 1) * 128],
                                         rhs=hT[:, hb_, k, 0:W], start=(k == 0), stop=(k == 7))
                        return r
                    P.add("pe", mmx, reads=["hT%d" % hb_] + ["w_in%d" % k_ for k_ in range(8)], writes=["ps%d" % bk])

                    on_pool = False
                    if on_pool:
                        P.add("act", lambda e, bk=bk, W=W: e.activation(out=ucp[:, 0:W], in_=psb(bk)[:, 0:W], func=AF.Copy),
                              reads=["ps%d" % bk], writes=["ucp"])
                        ca_v = cacc2[:, 0:W].rearrange("p (r w) -> p r w", w=RW)
                        u_v = ucp[:, 0:W].rearrange("p (r w) -> p r w", w=RW)
                        ceng, crd, cwr, csrc = "pool", ["ucp", "cw"], ["cacc2"], cacc2[:, 0:W]
                        fns = [lambda e, ca_v=ca_v, u_v=u_v, cc=cc: e.tensor_scalar(out=ca_v, in0=u_v, scalar1=cw[:, cc, 2:3], scalar2=0.0,
                                                                                  op0=ALU.mult, op1=ALU.add)]
                    else:
                        ca_v = cacc[:, cc % 2, 0:W].rearrange("p (r w) -> p r w", w=RW)
                        u_v = psb(bk)[:, 0:W].rearrange("p (r w) -> p r w", w=RW)
                        ceng, crd, cwr, csrc = "dve", ["ps%d" % bk, "cw"], ["cacc%d" % (cc % 2)], cacc[:, cc % 2, 0:W]
                        fns = [lambda e, ca_v=ca_v, u_v=u_v, cc=cc: e.tensor_scalar(out=ca_v, in0=u_v, scalar1=cw[:, cc, 2:3], scalar2=None, op0=ALU.mult)]
                    for t in (1, 3, 0, 4):
                        d_ = t - 2
                        if d_ < 0:
                            o = ca_v[:, :, -d_:RW]
                            i0 = u_v[:, :, 0:RW + d_]
                        else:
                            o = ca_v[:, :, 0:RW - d_]
                            i0 = u_v[:, :, d_:RW]
                        fns.append(lambda e, o=o, i0=i0, cc=cc, t=t: e.scalar_tensor_tensor(out=o, in0=i0, scalar=cw[:, cc, t:t + 1], in1=o,
                                                                                         op0=ALU.mult, op1=ALU.add))
                    seq(ceng, fns, crd, cwr)
                    P.add("act", lambda e, cc=cc, slot=slot, W=W, csrc=csrc: e.activation(out=xbcT[:, slot, 0:W], in_=csrc,
                                                                                          func=AF.Silu, bias=cb[:, cc:cc + 1]),
                          reads=cwr + ["cw"], writes=["xbcT%d" % slot])
                    if cc == 15:
                        for s in range(nsub):
                            b = subc[0] % 2
                            subc[0] += 1
                            rows = slice(srow + s * 128, srow + (s + 1) * 128)
                            transposes_list([xbcT[:, k, s * 128:(s + 1) * 128] for k in range(16)], 5,
                                            xto[:, b, :].rearrange("p (k t) -> p k t", t=128),
                                            ["xbcT%d" % k for k in range(16)], ["xto%d" % b])
                            dma("act", x_tok[rows, :], xto[:, b, :], ["xto%d" % b], ["x_tok_d"])
                    if cc == 23:
                        if not is_ctx:
                            dma("act", BT.rearrange("(g n) t -> n g t", n=128)[:, :, srow:srow + W], xbcT[:, 0:8, 0:W],
                                ["xbcT%d" % k for k in range(8)], ["BT_d"])
                        for s in range(nsub):
                            b = subc[0] % 2
                            subc[0] += 1
                            rows = slice(srow + s * 128, srow + (s + 1) * 128)
                            transposes_list([xbcT[:, k, s * 128:(s + 1) * 128] for k in range(8)], 5,
                                            bto[:, b, :].rearrange("p (k t) -> p k t", t=128),
                                            ["xbcT%d" % k for k in range(8)], ["bto%d" % b])
                            dma("act", B_tok[rows, :], bto[:, b, :], ["bto%d" % b], ["B_tok_d"])
                    if cc == 31:
                        dma("act", CT.rearrange("(g n) t -> n g t", n=128)[:, :, srow:srow + W], xbcT[:, 8:16, 0:W],
                            ["xbcT%d" % k for k in range(8, 16)], ["CT_d"])
            P.flush()

    def phase_C():
        with ExitStack() as es:
            def sb(name, shape, dt):
                return es.enter_context(nc.sbuf_tensor(name, shape, dt))
            Lex, Uex, ones, A_b = tri_consts(es)
            xt = sb("xtC", [128, 2, DI], BF16)
            bt = sb("btC", [128, 2, 1024], BF16)
            dtc = sb("dtcC", [128, 2, 64], F32)
            dA = sb("dAC", [128, 64], F32)
            exs = sb("exsC", [128, 128], F32)
            cf = sb("cfC", [128, 64], F32)
            xdtd = sb("xdtdC", [128, DI], BF16)
            hst = [sb("hFC", [128, DI], F32), sb("hBC", [128, DI], F32)]
            hbo = sb("hboC", [128, 2, DI], BF16)
            for d_ in range(2):
                P.add("pool", lambda e, d_=d_: e.memset(hst[d_][:], 0.0), writes=["h%d" % d_])
            cnt = [0]

            def step(row0, d_, store=None):
                b = cnt[0] % 2
                cnt[0] += 1
                rows = slice(row0, row0 + 128)
                dma("sp", xt[:, b, :], x_tok[rows, :], ["x_tok_d"], ["xt%d" % b])
                dma("sp", bt[:, b, :], B_tok[rows, :], ["B_tok_d"], ["bt%d" % b])
                dma("sp", dtc[:, b, :], dts[rows, :], ["dts_d"], ["dtc%d" % b])
                P.add("dve", lambda e: e.tensor_tensor(out=dA[:], in0=dtc[:, b, :], in1=A_b[:], op=ALU.mult),
                      reads=["dtc%d" % b, "A_b"], writes=["dA"])
                sl = slice(d_ * 32, (d_ + 1) * 32)
                tri = Uex if d_ == 0 else Lex

                def mms(e):
                    e.matmul(ps[:, 0, 0:32], lhsT=tri[:], rhs=dA[:, sl], start=True, stop=True)
                    return e.matmul(ps[:, 0, 32:64], lhsT=ones[:], rhs=dA[:, sl], start=True, stop=True)
                P.add("pe", mms, reads=["dA", "tri"], writes=["ps0"])
                P.add("act", lambda e: e.activation(out=exs[:, 0:64], in_=ps[:, 0, 0:64], func=AF.Exp),
                      reads=["ps0"], writes=["exs"])
                P.add("dve", lambda e: e.tensor_tensor(out=cf[:, 0:32], in0=dtc[:, b, sl], in1=exs[:, 0:32], op=ALU.mult),
                      reads=["dtc%d" % b, "exs"], writes=["cf"])
                P.add("dve", lambda e: e.tensor_tensor(out=bview(xdtd[:], 32, 64), in0=bview(xt[:, b, :], 32, 64),
                                                       in1=cf[:, 0:32].unsqueeze(2).to_broadcast([128, 32, 64]), op=ALU.mult),
                      reads=["xt%d" % b, "cf"], writes=["xdtd"])

                def mmst(e):
                    for g in range(8):
                        r = e.matmul(ps[:, 4 + g // 2, (g % 2) * 256:(g % 2 + 1) * 256], lhsT=bt[:, b, g * 128:(g + 1) * 128],
                                     rhs=xdtd[:, g * 256:(g + 1) * 256], start=True, stop=True)
                    return r
                P.add("pe", mmst, reads=["bt%d" % b, "xdtd"], writes=["ps4", "ps5", "ps6", "ps7"])
                h = hst[d_]
                if store is not None:
                    ob = store % 2
                    P.add("act", lambda e: e.activation(out=hbo[:, ob, :], in_=h[:], func=AF.Copy),
                          reads=["h%d" % d_], writes=["hbo%d" % ob])
                    dma("sp", hb[store], hbo[:, ob, :], ["hbo%d" % ob], ["hb_d"])
                P.add("dve", lambda e: e.tensor_tensor(out=bview(h[:], 32, 64), in0=bview(h[:], 32, 64),
                                                       in1=exs[:, 32:64].unsqueeze(2).to_broadcast([128, 32, 64]), op=ALU.mult),
                      reads=["exs", "h%d" % d_], writes=["h%d" % d_])
                P.add("dve", lambda e: e.tensor_tensor(out=h[:], in0=ps[:, 4:8, :].rearrange("p b n -> p (b n)"), in1=h[:], op=ALU.add),
                      reads=["ps4", "ps5", "ps6", "ps7", "h%d" % d_], writes=["h%d" % d_])

            step(T, 0)
            step(T + 128, 0)
            step(T + 128, 1)
            step(T, 1)
            for c in range(NCH - 1, -1, -1):
                step(c * 128, 1, store=c)
            dma("sp", hf0[:, :], hst[0][:], ["h0"], ["hf0_d"])
            P.flush()

    def phase_D():
        with ExitStack() as es:
            def sb(name, shape, dt):
                return es.enter_context(nc.sbuf_tensor(name, shape, dt))
            Lex, Uex, ones, A_b = tri_consts(es)
            negF = sb("negF", [128, 4, 128], BF16)
            negB = sb("negB", [128, 4, 128], BF16)
            D_b = sb("D_b", [128, 32], F32)
            DI = sb("DID", [128, 32, 128], F32)
            nw = sb("nwD", [128, 2048], F32)
            G1 = sb("G1D", [128, D], F32)
            w_out = sb("w_outD", [128, 16, D], BF16)
            xt = sb("xtD", [128, 2, 2048], BF16)
            btk = sb("btkD", [128, 2, 1024], BF16)
            BTc = sb("BTcD", [128, 2, 8, 128], BF16)
            CTc = sb("CTcD", [128, 2, 8, 128], BF16)
            szc = sb("szcD", [128, 2, 2048], BF16)
            dtc = sb("dtcD", [128, 2, 64], F32)
            hbc = sb("hbcD", [128, 2048], BF16)
            xres = sb("xresD", [128, 2, D], F32)
            dA = sb("dAD", [128, 64], F32)
            lndt = sb("lndtD", [128, 64], F32)
            acum = sb("acumD", [128, 2, 64], F32)
            nacum = sb("nacumD", [128, 2, 64], F32)
            ea = sb("eaD", [128, 64], F32)
            exs = sb("exsD", [128, 64], F32)
            cf = sb("cfD", [128, 32], F32)
            xdtd = sb("xdtdD", [128, 2048], BF16)
            hF = sb("hFD", [128, 2048], F32)
            hFb = sb("hFbD", [128, 2048], BF16)
            cbs = sb("cbsD", [128, 2, 8, 128], F32)
            ET = sb("ETD", [128, 4, 512], F32)
            MT = sb("MTD", [128, 2, 2, 512], BF16)
            MF = sb("MFD", [128, 2, 512], F32)
            ty = sb("tyD", [128, 2, 2048], F32)
            tyb = sb("tybD", [128, 2048], F32)
            ysb = sb("ysbD", [128, 2, 2048], F32)
            sq = sb("sqD", [128, 2048], F32)
            yn = sb("ynD", [128, 2048], BF16)
            ynT = sb("ynTD", [128, 16, 128], BF16)
            junk = sb("junkD", [128, D], BF16)
            st = sb("stD", [128, 8], F32)
            gst = sb("gstD", [128, 16], F32)

            fns = [lambda e: e.memset(negF[:], NEG), lambda e: e.memset(negB[:], NEG)]
            for j in range(4):
                fns.append(lambda e, j=j: e.affine_select(out=negF[:, j, :], in_=negF[:, j, :], pattern=[[-1, 128]],
                                                          compare_op=ALU.is_gt, fill=0.0, base=0, channel_multiplier=1))
                fns.append(lambda e, j=j: e.affine_select(out=negB[:, j, :], in_=negB[:, j, :], pattern=[[1, 128]],
                                                          compare_op=ALU.is_gt, fill=0.0, base=0, channel_multiplier=-1))
            seq("pool", fns, [], ["neg"])
            dma("sp", D_b[:], ssd_d[0:1, :].partition_broadcast(128), [], ["D_b"])
            for h in range(32):
                P.add("dve", lambda e, h=h: e.tensor_scalar(out=DI[:, h, :], in0=identf[:], scalar1=D_b[:, h:h + 1], scalar2=None, op0=ALU.mult),
                      reads=["D_b", "ident"], writes=["DI"])
            dma("sp", nw[:], ssd_norm[0:1, :].partition_broadcast(128), [], ["nw"])
            load_mod(0, 0, 0, None, None, G1)
            for k in range(4):
                dma("pool", w_out[:, k * 4:(k + 1) * 4, :],
                    ssd_w_out[k * 512:(k + 1) * 512, :].rearrange("(c p) d -> p c d", p=128), [], ["w_out%d" % k])
            dma("sp", hF[:], hf0[:, :], ["hf0_d"], ["hF"])

            def front_loads(c):
                b = c % 2
                rows = slice(c * 128, (c + 1) * 128)
                dma("sp", dtc[:, b, :], dts[rows, :], ["dts_d"], ["dtc%d" % b])
                dma("sp", xt[:, b, :], x_tok[rows, :], ["x_tok_d"], ["xt%d" % b])
                dma("sp", CTc[:, b], CT.rearrange("(g n) t -> n g t", n=128)[:, :, rows], ["CT_d"], ["CTc%d" % b])
                dma("sp", hbc[:], hb[c], ["hb_d"], ["hbc"])
                dma("sp", BTc[:, b], BT.rearrange("(g n) t -> n g t", n=128)[:, :, rows], ["BT_d"], ["BTc%d" % b])
                dma("sp", btk[:, b, :], B_tok[rows, :], ["B_tok_d"], ["btk%d" % b])

            def front(c):
                b = c % 2
                rows = slice(c * 128, (c + 1) * 128)
                dma("sp", szc[:, b, :], sz[rows, :], ["sz_d"], ["szc%d" % b])
                P.add("dve", lambda e: e.tensor_tensor(out=dA[:], in0=dtc[:, b, :], in1=A_b[:], op=ALU.mult),
                      reads=["dtc%d" % b, "A_b"], writes=["dA"])
                P.add("act", lambda e: e.activation(out=lndt[:], in_=dtc[:, b, :], func=AF.Ln), reads=["dtc%d" % b], writes=["lndt"])
                P.add("act", lambda e: e.activation(out=hFb[:], in_=hF[:], func=AF.Copy), reads=["hF"], writes=["hFb"])
                yield

                def mms(e):
                    e.matmul(ps[:, 6, 0:32], lhsT=Lex[:], rhs=dA[:, 0:32], start=True, stop=True)
                    e.matmul(ps[:, 6, 32:64], lhsT=Uex[:], rhs=dA[:, 32:64], start=True, stop=True)
                    e.matmul(ps[:, 6, 64:96], lhsT=Uex[:], rhs=dA[:, 0:32], start=True, stop=True)
                    return e.matmul(ps[:, 6, 96:128], lhsT=ones[:], rhs=dA[:, 0:32], start=True, stop=True)
                P.add("pe", mms, reads=["dA", "tri"], writes=["ps6"])
                yield
                P.add("dve", lambda e: e.tensor_tensor(out=acum[:, b, :], in0=ps[:, 6, 0:64], in1=dA[:], op=ALU.add),
                      reads=["ps6", "dA"], writes=["acum%d" % b])
                P.add("act", lambda e: e.activation(out=exs[:], in_=ps[:, 6, 64:128], func=AF.Exp), reads=["ps6"], writes=["exs"])
                yield
                P.add("dve", lambda e: e.tensor_tensor(out=nacum[:, b, :], in0=lndt[:], in1=acum[:, b, :], op=ALU.subtract),
                      reads=["acum%d" % b, "lndt"], writes=["nacum%d" % b])
                P.add("act", lambda e: e.activation(out=ea[:], in_=acum[:, b, :], func=AF.Exp), reads=["acum%d" % b], writes=["ea"])
                P.add("dve", lambda e: e.tensor_tensor(out=cf[:], in0=dtc[:, b, 0:32], in1=exs[:, 0:32], op=ALU.mult),
                      reads=["dtc%d" % b, "exs"], writes=["cf"])
                yield
                P.add("dve", lambda e: e.tensor_tensor(out=bview(xdtd[:], 32, 64), in0=bview(xt[:, b, :], 32, 64),
                                                       in1=cf[:].unsqueeze(2).to_broadcast([128, 32, 64]), op=ALU.mult),
                      reads=["xt%d" % b, "cf"], writes=["xdtd"])
                yield

                def scale(g):
                    for d_ in range(2):
                        tb = ty[:, b, :] if d_ == 0 else tyb[:]
                        P.add("dve", lambda e, g=g, d_=d_, tb=tb: e.tensor_tensor(
                            out=bview(tb[:, g * 256:(g + 1) * 256], 4, 64),
                            in0=bview(ps[:, 7, d_ * 256:(d_ + 1) * 256], 4, 64),
                            in1=ea[:, d_ * 32 + g * 4:d_ * 32 + (g + 1) * 4].unsqueeze(2).to_broadcast([128, 4, 64]), op=ALU.mult),
                            reads=["ps7", "ea"], writes=["ty%d" % b if d_ == 0 else "tyb"])
                for g in range(8):
                    def mmoff(e, g=g):
                        e.matmul(ps[:, 7, 0:256], lhsT=CTc[:, b, g, :], rhs=hFb[:, g * 256:(g + 1) * 256], start=True, stop=True)
                        return e.matmul(ps[:, 7, 256:512], lhsT=CTc[:, b, g, :], rhs=hbc[:, g * 256:(g + 1) * 256],
                                        start=True, stop=True)
                    P.add("pe", mmoff, reads=["CTc%d" % b, "hFb", "hbc"], writes=["ps7"])
                    yield
                    scale(g)
                yield

                def hupd(gp):
                    cs = slice(gp * 512, (gp + 1) * 512)
                    P.add("dve", lambda e, gp=gp, cs=cs: e.tensor_tensor(
                        out=bview(hF[:, cs], 8, 64), in0=bview(hF[:, cs], 8, 64),
                        in1=exs[:, 32 + gp * 8:32 + (gp + 1) * 8].unsqueeze(2).to_broadcast([128, 8, 64]), op=ALU.mult),
                        reads=["hF", "hFb", "exs"], writes=["hF"])
                    P.add("dve", lambda e, cs=cs: e.tensor_tensor(out=hF[:, cs], in0=ps[:, 6, :], in1=hF[:, cs], op=ALU.add),
                          reads=["ps6", "hF"], writes=["hF"])
                for gp in range(4):
                    def mmst(e, gp=gp):
                        for g in (2 * gp, 2 * gp + 1):
                            r = e.matmul(ps[:, 6, (g % 2) * 256:(g % 2 + 1) * 256], lhsT=btk[:, b, g * 128:(g + 1) * 128],
                                         rhs=xdtd[:, g * 256:(g + 1) * 256], start=True, stop=True)
                        return r
                    P.add("pe", mmst, reads=["btk%d" % b, "xdtd"], writes=["ps6"])
                    yield
                    hupd(gp)
                P.add("pool", lambda e: e.tensor_tensor(out=ty[:, b, :], in0=ty[:, b, :], in1=tyb[:], op=ALU.add),
                      reads=["ty%d" % b, "tyb"], writes=["ty%d" % b])
                yield

                def mmcb(e):
                    for g in range(8):
                        r = e.matmul(ps[:, g // 4, (g % 4) * 128:(g % 4 + 1) * 128], lhsT=BTc[:, b, g, :], rhs=CTc[:, b, g, :],
                                     start=True, stop=True)
                    return r
                P.add("pe", mmcb, reads=["BTc%d" % b, "CTc%d" % b], writes=["ps0", "ps1"])
                yield
                P.add("act", lambda e: e.activation(out=cbs[:, b].rearrange("p g t -> p (g t)"),
                                                    in_=ps[:, 0:2, :].rearrange("p b n -> p (b n)"), func=AF.Copy),
                      reads=["ps0", "ps1"], writes=["cbs%d" % b])
                yield

            rc = [0]

            def mid(c, side, nside):
                b = c % 2

                def diag(g):
                    yb = 4 + (g // 2) % 2

                    def mmdiag(e, g=g, yb=yb):
                        first = (g % 2 == 0)
                        for d_ in range(2):
                            for j in range(4):
                                hh = g * 4 + j
                                c0 = (g % 2) * 256 + j * 64
                                r = e.matmul(ps[:, yb, c0:c0 + 64], lhsT=MT[:, g % 2, d_, j * 128:(j + 1) * 128],
                                             rhs=xt[:, b, hh * 64:(hh + 1) * 64],
                                             start=(first and d_ == 0 and j == 0), stop=(g % 2 == 1 and d_ == 1 and j == 3))
                        return r
                    P.add("pe", mmdiag, reads=["MT%d0" % (g % 2), "MT%d1" % (g % 2), "xt%d" % b], writes=["ps%d" % yb])
                    if g % 2 == 1:
                        cs = slice((g - 1) * 256, (g + 1) * 256)
                        P.add("dve", lambda e, yb=yb, cs=cs: e.tensor_tensor(out=ysb[:, b, cs], in0=ps[:, yb, :], in1=ty[:, b, cs], op=ALU.add),
                              reads=["ps%d" % yb, "ty%d" % b], writes=["ysb%d" % b])

                for g in range(8):
                    for d_ in range(2):
                        rb = 2 + rc[0] % 2
                        eb = rc[0] % 4
                        rc[0] += 1
                        neg = negF if d_ == 0 else negB

                        def mmR(e, g=g, d_=d_, rb=rb, neg=neg):
                            e.matmul(ps[:, rb, :], lhsT=identb[:], rhs=neg[:].rearrange("p j t -> p (j t)"), start=True, stop=False)
                            for j in range(4):
                                h = d_ * 32 + g * 4 + j
                                r = e.matmul(ps[:, rb, j * 128:(j + 1) * 128], lhsT=acum[:, b, h:h + 1].to_broadcast([128, 128]),
                                             rhs=identf[:], start=False, stop=(j == 3))
                            return r
                        P.add("pe", mmR, reads=["acum%d" % b, "neg", "ident"], writes=["ps%d" % rb])

                        def exps(e, g=g, d_=d_, rb=rb, eb=eb):
                            for j in range(4):
                                h = d_ * 32 + g * 4 + j
                                r = e.activation(out=ET[:, eb, j * 128:(j + 1) * 128], in_=ps[:, rb, j * 128:(j + 1) * 128],
                                                 func=AF.Exp, bias=nacum[:, b, h:h + 1])
                            return r
                        P.add("act", exps, reads=["ps%d" % rb, "nacum%d" % b], writes=["ET%d" % eb])
                        if d_ == 0:
                            P.add("dve", lambda e, g=g, eb=eb: e.tensor_tensor(
                                out=MF[:, g % 2, :].rearrange("p (j t) -> p j t", j=4),
                                in0=ET[:, eb, :].rearrange("p (j t) -> p j t", j=4),
                                in1=cbs[:, b, g, :].unsqueeze(1).to_broadcast([128, 4, 128]), op=ALU.mult),
                                reads=["ET%d" % eb, "cbs%d" % b], writes=["MF%d" % (g % 2)])
                            P.add("dve", lambda e, g=g: e.tensor_tensor(
                                out=MT[:, g % 2, 0, :], in0=MF[:, g % 2, :],
                                in1=DI[:, g * 4:(g + 1) * 4, :].rearrange("p j t -> p (j t)"), op=ALU.add),
                                reads=["MF%d" % (g % 2), "DI"], writes=["MT%d0" % (g % 2)])
                        else:
                            P.add("dve", lambda e, g=g, eb=eb: e.tensor_tensor(
                                out=MT[:, g % 2, 1, :].rearrange("p (j t) -> p j t", j=4),
                                in0=ET[:, eb, :].rearrange("p (j t) -> p j t", j=4),
                                in1=cbs[:, b, g, :].unsqueeze(1).to_broadcast([128, 4, 128]), op=ALU.mult),
                                reads=["ET%d" % eb, "cbs%d" % b], writes=["MT%d1" % (g % 2)])
                    if g >= 1:
                        diag(g - 1)
                    for _ in range(nside):
                        next(side, None)
                diag(7)
                for _ in side:
                    pass

            def finA(c):
                b = c % 2
                yv = ysb[:, b, :]
                yr = "ysb%d" % b
                P.add("dve", lambda e: e.tensor_tensor(out=yv, in0=yv, in1=szc[:, b, :], op=ALU.mult),
                      reads=[yr, "szc%d" % b], writes=[yr])
                yield
                P.add("act", lambda e: e.activation(out=sq[:], in_=yv, func=AF.Square), reads=[yr, "Dtmp"], writes=["Dtmp"])
                yield
                P.add("dve", lambda e: e.tensor_reduce(out=gst[:, 0:8], in_=bview(sq[:], 8, 256), axis=mybir.AxisListType.X, op=ALU.add),
                      reads=["Dtmp"], writes=["gst"])
                yield
                P.add("act", lambda e: e.activation(out=gst[:, 8:16], in_=gst[:, 0:8], func=AF.Sqrt, bias=EPS, scale=1.0 / 256),
                      reads=["gst"], writes=["gst2"])
                yield
                P.add("dve", lambda e: e.reciprocal(out=gst[:, 8:16], in_=gst[:, 8:16]), reads=["gst2"], writes=["gst2"])
                P.add("dve", lambda e: e.tensor_tensor(out=bview(yv, 8, 256), in0=bview(yv, 8, 256),
                                                       in1=gst[:, 8:16].unsqueeze(2).to_broadcast([128, 8, 256]), op=ALU.mult),
                      reads=["gst2", yr], writes=[yr])
                yield
                P.add("pool", lambda e: e.tensor_tensor(out=yn[:], in0=yv, in1=nw[:], op=ALU.mult),
                      reads=[yr, "nw"], writes=["yn"])
                yield

            def finB(c):
                b = c % 2
                rows = slice(c * 128, (c + 1) * 128)
                dma("sp", xres[:, b, :], x_in[rows, :], [], ["xres%d" % b])

                def tr(e):
                    for k in range(16):
                        r = e.transpose(out=psb16(k // 8)[:, (k % 8) * 128:(k % 8 + 1) * 128], in_=yn[:, k * 128:(k + 1) * 128],
                                        identity=identb[:])
                    return r
                P.add("pe", tr, reads=["yn", "ident"], writes=["ps0", "ps1"])
                yield
                for i in range(2):
                    P.add("act", lambda e, i=i: e.activation(out=ynT[:, i * 8:(i + 1) * 8, :],
                                                             in_=psb16(i).rearrange("p (k t) -> p k t", t=128), func=AF.Copy),
                          reads=["ps%d" % i], writes=["ynT"])
                yield

                def mmo(e):
                    for half in range(2):
                        for k in range(16):
                            r = e.matmul(ps[:, 6 + half, :], lhsT=ynT[:, k, :], rhs=w_out[:, k, half * 512:(half + 1) * 512],
                                         start=(k == 0), stop=(k == 15))
                    return r
                P.add("pe", mmo, reads=["ynT"] + ["w_out%d" % k_ for k_ in range(4)], writes=["ps6", "ps7"])
                yield
                y_ap = ps[:, 6:8, :].rearrange("p b n -> p (b n)")
                P.add("act", lambda e: e.activation(out=junk[:], in_=y_ap, func=AF.Square, accum_out=st[:, 2:3]),
                      reads=["ps6", "ps7"], writes=["Dpjunk", "Dpss"])
                yield
                P.add("act", lambda e: e.activation(out=st[:, 3:4], in_=st[:, 2:3], func=AF.Sqrt, bias=EPS, scale=1.0 / D),
                      reads=["Dpss"], writes=["Dprs"])
                yield
                P.add("dve", lambda e: e.reciprocal(out=st[:, 3:4], in_=st[:, 3:4]), reads=["Dprs"], writes=["Dprs"])
                P.add("dve", lambda e: e.scalar_tensor_tensor(out=sq[:, 0:D], in0=y_ap, scalar=st[:, 3:4], in1=G1[:],
                                                              op0=ALU.mult, op1=ALU.mult),
                      reads=["ps6", "ps7", "Dprs", "modtiles"], writes=["Dtmp"])
                yield
                P.add("pool", lambda e: e.tensor_tensor(out=xres[:, b, :], in0=xres[:, b, :], in1=sq[:, 0:D], op=ALU.add),
                      reads=["Dtmp", "xres%d" % b], writes=["xres%d" % b])
                yield
                dma("sp", x1[rows, :], xres[:, b, :], ["xres%d" % b], ["x1_d"])
                yield

            import itertools
            front_loads(0)
            if d_stop >= 1:
                for _ in front(0):
                    pass
            for c in range(NCH if d_stop >= 2 else 0):
                if d_stop == 2 and c >= 1:
                    break
                if d_stop == 3 and c >= 2:
                    break
                if c + 1 < NCH:
                    front_loads(c + 1)
                gens = []
                if c >= 1:
                    gens += [finA(c - 1), finB(c - 1)]
                if c + 1 < NCH:
                    gens.append(front(c + 1))
                mid(c, itertools.chain(*gens), NSIDE)
            if d_stop >= 4:
                for _ in itertools.chain(finA(NCH - 1), finB(NCH - 1)):
                    pass
            P.flush()

    def bdm(bd, half):
        return bd + half

    I32 = mybir.dt.int32
    hs = dt_("hs", [NE * T, 512], F32).ap()
    ys = dt_("ys", [NE * T, D], F32).ap()
    x2 = dt_("x2", [T, D], F32).ap()
    NI = T // 512
    ctab = dt_("ctab", [1, NE * NI], I32).ap()

    def phase_E():
        NT = T // 1024
        NS = T // 128
        with ExitStack() as es0:
            def sb0(name, shape, dt):
                return es0.enter_context(nc.sbuf_tensor(name, shape, dt))
            idx = [[sb0("idx%d_%d" % (k, n), [128, 1], I32) for n in range(NS)] for k in range(2)]
            gts = sb0("gtsE", [128, NS, 2], F32)
            carry = sb0("carryE", [128, NE], F32)
            P.add("pool", lambda e: e.memset(carry[:], 0.0), writes=["carry"])

            with ExitStack() as es:
                def sb(name, shape, dt):
                    return es.enter_context(nc.sbuf_tensor(name, shape, dt))
                xres = sb("xresE", [128, 8, D], F32)
                hT = sb("hTE", [128, 8, 1024], BF16)
                acc = sb("accE", [128, 8, D], F32)
                aT = sb("aTE", [128, 2, 4, 1024], BF16)
                wgu = sb("wguE", [128, 2, 8, 2, 512], BF16)
                wdn = sb("wdnE", [128, 2, 4, D], BF16)
                At = sb("AtE", [128, D], F32)
                Bt = sb("BtE", [128, D], F32)
                Gt = sb("GtE", [128, D], F32)
                tmp = sb("tmpE", [128, D], F32)
                tmpb = sb("tmpbE", [128, D], F32)
                tmps = [tmp, tmpb]
                junk = sb("junkE", [128, D], F32)
                st = sb("stE", [128, 16], F32)
                hn = sb("hnE", [128, 2, D], BF16)
                hn32 = sb("hn32E", [128, D], F32)
                stmp = sb("stmpE", [128, 2, 512], F32)
                uu = sb("uuE", [128, 2, 512], F32)
                ca = sb("caE", [128, 2, 512], F32)
                scw3 = sb("scw3E", [128, 8, 3], F32)
                h32T = sb("h32TE", [128, 8, 128], F32)
                rt = sb("rtE", [128, 8, NE], F32)
                lg = sb("lgE", [128, 6, NE], F32)
                sm = sb("smE", [128, 12], F32)
                Lex = sb("LexE", [128, 128], F32)
                ones = sb("onesE", [128, 128], F32)
                ebase = sb("ebaseE", [128, NE], F32)
                seq("pool", [
                    lambda e: e.memset(ones[:], 1.0),
                    lambda e: e.memset(Lex[:], 1.0),
                    lambda e: e.affine_select(out=Lex[:], in_=Lex[:], pattern=[[1, 128]], compare_op=ALU.is_gt, fill=0.0,
                                              base=0, channel_multiplier=-1),
                ], [], ["tri"])
                for ex in range(NE):
                    P.add("pool", lambda e, ex=ex: e.memset(ebase[:, ex:ex + 1], float(ex * T)), writes=["ebase"])
                for t in range(3):
                    dma_slow("sp", scw3[:, :, t], sc_conv_w[t].rearrange("(c p) -> p c", p=128), [], ["scw3"])
                dma("sp", rt[:], moe_router.rearrange("(k p) e -> p k e", p=128), [], ["rt"])
                cnt = dict(blk=0, gu=0, dn=0, sc=0)

                pending = [None]

                def ffn_dense():
                    F = DFF
                    nfc = F // 128
                    wv = ffn_w_gu.rearrange("(k p) n -> p k n", p=128)
                    fc0 = 0
                    bi = 0
                    while fc0 < nfc:
                        nb = min(4, nfc - fc0)
                        b = cnt["blk"] % 2
                        cnt["blk"] += 1
                        dma("pool", wgu[:, b, :, 0, 0:nb * 128], wv[:, :, fc0 * 128:(fc0 + nb) * 128], [], ["wgug%d" % b])
                        dma("pool", wgu[:, b, :, 1, 0:nb * 128], wv[:, :, F + fc0 * 128:F + (fc0 + nb) * 128], [], ["wguu%d" % b])
                        dma("pool", wdn[:, b, 0:nb, :], ffn_w_down[fc0 * 128:(fc0 + nb) * 128, :].rearrange("(c p) d -> p c d", p=128),
                            [], ["wdn%d" % b])
                        for j in range(nb):
                            for th in range(2):
                                q = cnt["gu"] % 2
                                cnt["gu"] += 1
                                bg = q * 2
                                bu = q * 2 + 1

                                def mmgu(e, b=b, j=j, th=th, bg=bg, bu=bu):
                                    for k in range(8):
                                        e.matmul(psb(bg), lhsT=wgu[:, b, k, 0, j * 128:(j + 1) * 128], rhs=hT[:, k, th * 512:(th + 1) * 512],
                                                 start=(k == 0), stop=(k == 7))
                                    for k in range(8):
                                        r = e.matmul(psb(bu), lhsT=wgu[:, b, k, 1, j * 128:(j + 1) * 128], rhs=hT[:, k, th * 512:(th + 1) * 512],
                                                     start=(k == 0), stop=(k == 7))
                                    return r
                                P.add("pe", mmgu, reads=["wgug%d" % b, "wguu%d" % b, "hT"], writes=["ps%d" % bg, "ps%d" % bu])
                                P.add("act", lambda e, q=q, bg=bg: e.activation(out=stmp[:, q, :], in_=psb(bg), func=AF.Silu),
                                      reads=["ps%d" % bg], writes=["stmp%d" % q])
                                P.add("dve", lambda e, q=q, bu=bu, b=b, j=j, th=th: e.tensor_tensor(
                                    out=aT[:, b, j, th * 512:(th + 1) * 512], in0=psb(bu), in1=stmp[:, q, :], op=ALU.mult),
                                    reads=["ps%d" % bu, "stmp%d" % q], writes=["aT%d" % b])
                        def down(b=b, nb=nb, bi=bi):
                            for s in range(8):
                                q = cnt["dn"] % 2
                                cnt["dn"] += 1
                                bd = 4 + q * 2

                                def mmdn(e, b=b, s=s, bd=bd, nb=nb):
                                    for half in range(2):
                                        for j in range(nb):
                                            r = e.matmul(psb(bd + half), lhsT=aT[:, b, j, s * 128:(s + 1) * 128],
                                                         rhs=wdn[:, b, j, half * 512:(half + 1) * 512], start=(j == 0), stop=(j == nb - 1))
                                    return r
                                P.add("pe", mmdn, reads=["aT%d" % b, "wdn%d" % b], writes=["ps%d" % bd, "ps%d" % (bd + 1)])
                                srcp = ps[:, bd:bd + 2, :].rearrange("p b n -> p (b n)")
                                rd = ["ps%d" % bd, "ps%d" % (bd + 1)]
                                if bi == 0:
                                    P.add("dve", lambda e, s=s, srcp=srcp: e.tensor_copy(out=acc[:, s, :], in_=srcp),
                                          reads=rd, writes=["acc%d" % s])
                                else:
                                    P.add("dve", lambda e, s=s, srcp=srcp: e.tensor_tensor(out=acc[:, s, :], in0=srcp, in1=acc[:, s, :], op=ALU.add),
                                          reads=rd + ["acc%d" % s], writes=["acc%d" % s])
                        if pending[0] is not None:
                            pending[0]()
                        pending[0] = down
                        fc0 += nb
                        bi += 1
                    pending[0]()
                    pending[0] = None

                XA = mybir.AxisListType.X

                def norm_phase():
                    def nm(s):
                        return norm_mod(xres[:, s, :], ["xres%d" % s], At, Bt, hn[:, s % 2, :], tmps[s % 2], junk, st, "E", par=s % 2)
                    hres = nm(0)
                    for s in range(8):
                        nxt = nm(s + 1) if s + 1 < 8 else None
                        transposes_list([hn[:, s % 2, k * 128:(k + 1) * 128] for k in range(8)], 7,
                                        hT[:, :, s * 128:(s + 1) * 128], [hres], ["hT"])
                        hres = nxt

                for tt in range(NT):
                    t0 = tt * 1024
                    for s in range(8):
                        dma("sp", xres[:, s, :], x1[t0 + s * 128:t0 + (s + 1) * 128, :], ["x1_d"], ["xres%d" % s])
                    load_mod(0, 1, 0, At, Bt, Gt)
                    norm_phase()
                    ffn_dense()
                    for s in range(8):
                        post_norm_res(acc[:, s, :], ["acc%d" % s], Gt, xres[:, s, :], ["xres%d" % s], tmps[s % 2], junk, st, "E", par=s % 2)
                    if dbg:
                        for s in range(8):
                            dma("sp", dbg_ffn[t0 + s * 128:t0 + (s + 1) * 128, :], xres[:, s, :], ["xres%d" % s], ["dbg_ffn"])
                    load_mod(1, 0, 0, At, Bt, Gt)
                    norm_phase()
                    for k2 in range(2):
                        dma("pool", wdn[:, k2, :, :], sc_w_out[k2 * 512:(k2 + 1) * 512, :].rearrange("(c p) d -> p c d", p=128),
                            [], ["wdn%d" % k2])
                    scv = sc_w_in.rearrange("(k p) (j c) -> p k j c", p=128, j=3)
                    for i in range(8):
                        b = cnt["blk"] % 2
                        cnt["blk"] += 1
                        scw = wgu[:, b].rearrange("p k j c -> p (k j c)")[:, 0:3072].rearrange("p (k j c) -> p k j c", k=8, j=3)
                        for j3 in range(3):
                            dma("pool", scw[:, :, j3, :], scv[:, :, j3, i * 128:(i + 1) * 128], [], ["wgug%d" % b, "wguu%d" % b])
                        for th in range(2):
                            q = cnt["sc"] % 2
                            cnt["sc"] += 1
                            B0 = q * 4

                            def mmsc(e, scw=scw, th=th, B0=B0):
                                for j in range(3):
                                    for k in range(8):
                                        r = e.matmul(psb(B0 + j), lhsT=scw[:, k, j, :], rhs=hT[:, k, th * 512:(th + 1) * 512],
                                                     start=(k == 0), stop=(k == 7))
                                return r
                            P.add("pe", mmsc, reads=["wgug%d" % b, "wguu%d" % b, "hT"], writes=["ps%d" % (B0 + j) for j in range(3)])
                            P.add("act", lambda e, q=q, B0=B0: e.activation(out=stmp[:, q, :], in_=psb(B0 + 2), func=AF.Copy),
                                  reads=["ps%d" % (B0 + 2)], writes=["stmp%d" % q])
                            P.add("dve", lambda e, q=q, B0=B0: e.tensor_tensor(out=uu[:, q, :], in0=psb(B0 + 1), in1=stmp[:, q, :], op=ALU.mult),
                                  reads=["ps%d" % (B0 + 1), "stmp%d" % q], writes=["uu%d" % q])
                            cv = ca[:, q, :].rearrange("p (r w) -> p r w", w=64)
                            uv = uu[:, q, :].rearrange("p (r w) -> p r w", w=64)
                            seq("dve", [
                                lambda e, cv=cv, uv=uv, i=i: e.tensor_scalar(out=cv, in0=uv, scalar1=scw3[:, i, 1:2], scalar2=None, op0=ALU.mult),
                                lambda e, cv=cv, uv=uv, i=i: e.scalar_tensor_tensor(out=cv[:, :, 1:64], in0=uv[:, :, 0:63], scalar=scw3[:, i, 0:1],
                                                                                  in1=cv[:, :, 1:64], op0=ALU.mult, op1=ALU.add),
                                lambda e, cv=cv, uv=uv, i=i: e.scalar_tensor_tensor(out=cv[:, :, 0:63], in0=uv[:, :, 1:64], scalar=scw3[:, i, 2:3],
                                                                                  in1=cv[:, :, 0:63], op0=ALU.mult, op1=ALU.add),
                            ], ["uu%d" % q, "scw3"], ["ca%d" % q])
                            P.add("dve", lambda e, q=q, B0=B0, i=i, th=th: e.tensor_tensor(
                                out=aT[:, i // 4, i % 4, th * 512:(th + 1) * 512], in0=psb(B0), in1=ca[:, q, :], op=ALU.mult),
                                reads=["ps%d" % B0, "ca%d" % q], writes=["aT%d" % (i // 4)])
                    for s in range(8):
                        q = cnt["dn"] % 2
                        cnt["dn"] += 1
                        bd = 4 + q * 2

                        def mmso(e, s=s, bd=bd):
                            for half in range(2):
                                for i in range(8):
                                    r = e.matmul(psb(bd + half), lhsT=aT[:, i // 4, i % 4, s * 128:(s + 1) * 128],
                                                 rhs=wdn[:, i // 4, i % 4, half * 512:(half + 1) * 512], start=(i == 0), stop=(i == 7))
                            return r
                        P.add("pe", mmso, reads=["aT0", "aT1", "wdn0", "wdn1"], writes=["ps%d" % bd, "ps%d" % (bd + 1)])
                        post_norm_res(ps[:, bd:bd + 2, :].rearrange("p b n -> p (b n)"), ["ps%d" % bd, "ps%d" % (bd + 1)], Gt,
                                      xres[:, s, :], ["xres%d" % s], tmps[s % 2], junk, st, "E", par=s % 2)
                        dma("sp", x2[t0 + s * 128:t0 + (s + 1) * 128, :], xres[:, s, :], ["xres%d" % s], ["x2_d"])
                    if dbg:
                        for s in range(8):
                            dma("sp", dbg_sc[t0 + s * 128:t0 + (s + 1) * 128, :], xres[:, s, :], ["xres%d" % s], ["dbg_sc"])
                    load_mod(1, 1, 0, At, Bt, None)
                    for s in range(8):
                        n = tt * 8 + s
                        hb_ = n % 2
                        norm_mod(xres[:, s, :], ["xres%d" % s], At, Bt, hn[:, hb_, :], tmp, junk, st, "E%d" % hb_, hn32=hn32[:])

                        def tr32(e):
                            for k in range(8):
                                r = e.transpose(out=ps[:, 4 + k // 4, (k % 4) * 128:(k % 4 + 1) * 128], in_=hn32[:, k * 128:(k + 1) * 128],
                                                identity=identf[:])
                            return r
                        P.add("pe", tr32, reads=["E%dhn32" % hb_, "ident"], writes=["ps4", "ps5"])
                        P.add("act", lambda e: e.activation(out=h32T[:].rearrange("p k t -> p (k t)"),
                                                            in_=ps[:, 4:6, :].rearrange("p b n -> p (b n)"), func=AF.Copy),
                              reads=["ps4", "ps5"], writes=["h32T"])

                        def mmr(e):
                            for k in range(8):
                                r = e.matmul(ps[:, 6, 0:NE], lhsT=h32T[:, k, :], rhs=rt[:, k, :], start=(k == 0), stop=(k == 7))
                            return r
                        P.add("pe", mmr, reads=["h32T", "rt"], writes=["ps6"])
                        l0 = lg[:, 0, :]
                        m1 = lg[:, 1, :]
                        l2 = lg[:, 2, :]
                        m2 = lg[:, 3, :]
                        m12 = lg[:, 4, :]
                        rk = lg[:, 5, :]
                        seq("dve", [
                            lambda e: e.tensor_copy(out=l0, in_=ps[:, 6, 0:NE]),
                            lambda e: e.tensor_reduce(out=sm[:, 0:1], in_=l0, axis=XA, op=ALU.max),
                            lambda e: e.tensor_scalar(out=m1, in0=l0, scalar1=sm[:, 0:1], scalar2=None, op0=ALU.is_equal),
                            lambda e: e.scalar_tensor_tensor(out=l2, in0=m1, scalar=-1e30, in1=l0, op0=ALU.mult, op1=ALU.add),
                            lambda e: e.tensor_reduce(out=sm[:, 1:2], in_=l2, axis=XA, op=ALU.max),
                            lambda e: e.tensor_scalar(out=m2, in0=l2, scalar1=sm[:, 1:2], scalar2=None, op0=ALU.is_equal),
                            lambda e: e.tensor_tensor(out=sm[:, 2:3], in0=sm[:, 1:2], in1=sm[:, 0:1], op=ALU.subtract),
                            lambda e: e.tensor_tensor(out=m12, in0=m1, in1=m2, op=ALU.add),
                        ], ["ps6"], ["lg"])
                        P.add("act", lambda e: e.activation(out=sm[:, 3:4], in_=sm[:, 2:3], func=AF.Exp), reads=["lg"], writes=["sm3"])
                        seq("dve", [
                            lambda e: e.tensor_scalar(out=sm[:, 4:5], in0=sm[:, 3:4], scalar1=1.0, scalar2=None, op0=ALU.add),
                            lambda e, n=n: e.reciprocal(out=gts[:, n, 0:1], in_=sm[:, 4:5]),
                            lambda e, n=n: e.tensor_tensor(out=gts[:, n, 1:2], in0=sm[:, 3:4], in1=gts[:, n, 0:1], op=ALU.mult),
                        ], ["sm3", "lg"], ["gts", "lg"])
                        def mmrank(e):
                            e.matmul(ps[:, 6, 64:64 + NE], lhsT=Lex[:], rhs=m12, start=True, stop=True)
                            return e.matmul(ps[:, 6, 128:128 + NE], lhsT=ones[:], rhs=m12, start=True, stop=True)
                        P.add("pe", mmrank, reads=["lg", "tri"], writes=["ps6"])
                        seq("dve", [
                            lambda e: e.tensor_tensor(out=rk, in0=ps[:, 6, 64:64 + NE], in1=carry[:], op=ALU.add),
                            lambda e: e.tensor_tensor(out=rk, in0=rk, in1=ebase[:], op=ALU.add),
                            lambda e: e.tensor_tensor(out=carry[:], in0=ps[:, 6, 128:128 + NE], in1=carry[:], op=ALU.add),
                            lambda e: e.tensor_tensor(out=l0, in0=rk, in1=m1, op=ALU.mult),
                            lambda e: e.tensor_reduce(out=sm[:, 6:7], in_=l0, axis=XA, op=ALU.add),
                            lambda e: e.tensor_tensor(out=l2, in0=rk, in1=m2, op=ALU.mult),
                            lambda e: e.tensor_reduce(out=sm[:, 7:8], in_=l2, axis=XA, op=ALU.add),
                            lambda e, n=n: e.tensor_copy(out=idx[0][n][:], in_=sm[:, 6:7]),
                            lambda e, n=n: e.tensor_copy(out=idx[1][n][:], in_=sm[:, 7:8]),
                        ], ["ps6", "carry", "ebase", "lg"], ["lg", "carry", "idx%d" % n])
                        for k in range(2):
                            P.add("pool", lambda e, k=k, n=n, hb_=hb_: e.indirect_dma_start(
                                out=hs[:, :], out_offset=bass.IndirectOffsetOnAxis(ap=idx[k][n][:, :], axis=0),
                                in_=hn[:, hb_, :].bitcast(F32), in_offset=None),
                                reads=["E%dhn" % hb_, "idx%d" % n], writes=["hs_d"], dma=True)
                ctf = sb("ctfE", [1, NE, NI], F32)
                cti = sb("ctiE", [1, NE, NI], I32)
                for i in range(NI):
                    P.add("dve", lambda e, i=i: e.tensor_scalar(out=ctf[:, :, i], in0=carry[0:1, :], scalar1=float(512 * i), scalar2=None,
                                                                op0=ALU.is_gt), reads=["carry"], writes=["ctf"])
                P.add("dve", lambda e: e.tensor_copy(out=cti[:], in_=ctf[:]), reads=["ctf"], writes=["cti"])
                dma("sp", ctab[:, :], cti[:].rearrange("o e i -> o (e i)"), ["cti"], ["ctab_d"])
                P.flush()

            if e_stop < 2:
                return
            with ExitStack() as es:
                def sb(name, shape, dt):
                    return es.enter_context(nc.sbuf_tensor(name, shape, dt))
                hsb = sb("hsbE2", [128, 2, 4, D], BF16)
                hT2 = sb("hTE2", [128, 2, 8, 512], BF16)
                aT = sb("aTE2", [128, 2, 4, 512], BF16)
                wgu = sb("wguE2", [128, 2, 8, 2, 512], BF16)
                wdn = sb("wdnE2", [128, 2, 4, D], BF16)
                yacc = sb("yaccE2", [128, 4, D], F32)
                stmp = sb("stmpE2", [128, 2, 512], F32)
                cnt = dict(blk=0, gu=0, dn=0, item=0)
                pend2 = [None]

                def wload(ex, bi):
                    wv = moe_w_gu[ex].rearrange("(k p) n -> p k n", p=128)
                    fc0 = bi * 4
                    b = cnt["blk"] % 2
                    cnt["blk"] += 1
                    dma("pool", wgu[:, b, :, 0, :], wv[:, :, fc0 * 128:(fc0 + 4) * 128], [], ["wgug%d" % b])
                    dma("pool", wgu[:, b, :, 1, :], wv[:, :, DFE + fc0 * 128:DFE + (fc0 + 4) * 128], [], ["wguu%d" % b])
                    dma("pool", wdn[:, b, :, :], moe_w_down[ex, fc0 * 128:(fc0 + 4) * 128, :].rearrange("(c p) d -> p c d", p=128),
                        [], ["wdn%d" % b])
                    return b

                def pf_rows(ex, it):
                    pb = cnt["item"] % 2
                    cnt["item"] += 1
                    r0 = ex * T + it * 512
                    dma("sp", hsb[:, pb].bitcast(F32), hs[r0:r0 + 512, :].rearrange("(s p) d -> p s d", p=128), ["hs_d"], ["hsb%d" % pb])
                    return pb

                def prefetch(ex, it, pb):
                    for s in range(4):
                        transposes_list([hsb[:, pb, s, k * 128:(k + 1) * 128] for k in range(8)], 7,
                                        hT2[:, pb, :, s * 128:(s + 1) * 128], ["hsb%d" % pb], ["hT%d" % pb])
                    b0 = wload(ex, 0)
                    return pb, b0

                for ex in range(NE):
                    nxt = prefetch(ex, 0, pf_rows(ex, 0))
                    for it in range(NI):
                        ci = ex * NI + it
                        P.begin_cond(ctab[0:1, ci:ci + 1], ["ctab_d"])
                        r0 = ex * T + it * 512
                        pb, b0 = nxt
                        if it + 1 < NI:
                            pbn = pf_rows(ex, it + 1)
                        hT = hT2[:, pb]
                        hTr = "hT%d" % pb
                        for bi in range(7):
                            b = b0 if bi == 0 else wload(ex, bi)
                            for j in range(4):
                                q = cnt["gu"] % 2
                                cnt["gu"] += 1
                                bg = q * 2
                                bu = q * 2 + 1

                                def mmgu(e, b=b, j=j, bg=bg, bu=bu, hT=hT):
                                    for k in range(8):
                                        e.matmul(psb(bg), lhsT=wgu[:, b, k, 0, j * 128:(j + 1) * 128], rhs=hT[:, k, :],
                                                 start=(k == 0), stop=(k == 7))
                                    for k in range(8):
                                        r = e.matmul(psb(bu), lhsT=wgu[:, b, k, 1, j * 128:(j + 1) * 128], rhs=hT[:, k, :],
                                                     start=(k == 0), stop=(k == 7))
                                    return r
                                P.add("pe", mmgu, reads=["wgug%d" % b, "wguu%d" % b, hTr], writes=["ps%d" % bg, "ps%d" % bu])
                                P.add("act", lambda e, q=q, bg=bg: e.activation(out=stmp[:, q, :], in_=psb(bg), func=AF.Silu),
                                      reads=["ps%d" % bg], writes=["stmp%d" % q])
                                P.add("dve", lambda e, q=q, bu=bu, b=b, j=j: e.tensor_tensor(
                                    out=aT[:, b, j, :], in0=psb(bu), in1=stmp[:, q, :], op=ALU.mult),
                                    reads=["ps%d" % bu, "stmp%d" % q], writes=["aT%d" % b])
                            def down(b=b, bi=bi):
                                for s in range(4):
                                    q = cnt["dn"] % 2
                                    cnt["dn"] += 1
                                    bd = 4 if q == 0 else 6

                                    def mmdn(e, b=b, s=s, bd=bd):
                                        for half in range(2):
                                            for j in range(4):
                                                r = e.matmul(psb(bdm(bd, half)), lhsT=aT[:, b, j, s * 128:(s + 1) * 128],
                                                             rhs=wdn[:, b, j, half * 512:(half + 1) * 512], start=(j == 0), stop=(j == 3))
                                        return r
                                    P.add("pe", mmdn, reads=["aT%d" % b, "wdn%d" % b], writes=["ps%d" % bdm(bd, 0), "ps%d" % bdm(bd, 1)])
                                    for half in range(2):
                                        hs_ = slice(half * 512, (half + 1) * 512)
                                        if bi == 0:
                                            P.add("dve", lambda e, s=s, half=half, hs_=hs_, bd=bd: e.tensor_copy(out=yacc[:, s, hs_], in_=psb(bdm(bd, half))),
                                                  reads=["ps%d" % bdm(bd, half)], writes=["yacc%d" % s])
                                        else:
                                            P.add("dve", lambda e, s=s, half=half, hs_=hs_, bd=bd: e.tensor_tensor(
                                                out=yacc[:, s, hs_], in0=psb(bdm(bd, half)), in1=yacc[:, s, hs_], op=ALU.add),
                                                reads=["ps%d" % bdm(bd, half), "yacc%d" % s], writes=["yacc%d" % s])
                            if pend2[0] is not None:
                                pend2[0]()
                            pend2[0] = down
                        if it + 1 < NI:
                            nxt = prefetch(ex, it + 1, pbn)
                        pend2[0]()
                        pend2[0] = None
                        for s in range(4):
                            dma("sp", ys[r0 + s * 128:r0 + (s + 1) * 128, :], yacc[:, s, :], ["yacc%d" % s], ["ys_d"])
                        P.end_cond()
                P.flush()

            if e_stop < 3:
                return
            with ExitStack() as es:
                def sb(name, shape, dt):
                    return es.enter_context(nc.sbuf_tensor(name, shape, dt))
                Gt = sb("GtE3", [128, D], F32)
                NB3 = 4
                xr = sb("xrE3", [128, NB3, D], F32)
                y1 = sb("y1E3", [128, NB3, D], F32)
                y2 = sb("y2E3", [128, NB3, D], F32)
                tmp3 = [sb("tmpE3a", [128, D], F32), sb("tmpE3b", [128, D], F32)]
                junk = sb("junkE3", [128, D], BF16)
                st = sb("stE3", [128, 16], F32)
                load_mod(1, 1, 0, None, None, Gt)

                def loads(n):
                    b = n % NB3
                    rows = slice(n * 128, (n + 1) * 128)
                    dma("sp", xr[:, b, :], x2[rows, :], ["x2_d"], ["xr%d" % b])
                    P.add("pool", lambda e: e.indirect_dma_start(
                        out=y1[:, b, :], out_offset=None, in_=ys[:, :],
                        in_offset=bass.IndirectOffsetOnAxis(ap=idx[0][n][:, :], axis=0)),
                        reads=["ys_d"], writes=["y1%d" % b], dma=True)
                    P.add("pool", lambda e: e.indirect_dma_start(
                        out=y2[:, b, :], out_offset=None, in_=ys[:, :],
                        in_offset=bass.IndirectOffsetOnAxis(ap=idx[1][n][:, :], axis=0)),
                        reads=["ys_d"], writes=["y2%d" % b], dma=True)
                for n in range(min(NB3 - 1, NS)):
                    loads(n)
                for n in range(NS):
                    if n + NB3 - 1 < NS:
                        loads(n + NB3 - 1)
                    b = n % NB3
                    par = n % 2
                    c0 = 8 * par
                    rows = slice(n * 128, (n + 1) * 128)
                    P.add("dve", lambda e, n=n, b=b: e.tensor_scalar(out=y1[:, b, :], in0=y1[:, b, :], scalar1=gts[:, n, 0:1], scalar2=None,
                                                                     op0=ALU.mult), reads=["y1%d" % b], writes=["y1%d" % b])
                    P.add("dve", lambda e, n=n, b=b: e.scalar_tensor_tensor(out=y1[:, b, :], in0=y2[:, b, :], scalar=gts[:, n, 1:2], in1=y1[:, b, :],
                                                                            op0=ALU.mult, op1=ALU.add),
                          reads=["y1%d" % b, "y2%d" % b], writes=["y1%d" % b])
                    tg = "E3p%d" % par
                    rstd_ops(y1[:, b, :], junk[:], st[:, c0 + 2:c0 + 3], st[:, c0 + 3:c0 + 4], D, ["y1%d" % b], tg)
                    P.add("dve", lambda e, b=b, c0=c0, par=par: e.scalar_tensor_tensor(out=tmp3[par][:], in0=y1[:, b, :], scalar=st[:, c0 + 3:c0 + 4],
                                                                                     in1=Gt[:], op0=ALU.mult, op1=ALU.mult),
                          reads=["y1%d" % b, tg + "rs", "modtiles"], writes=[tg + "tmp"])
                    P.add("dve", lambda e, b=b, par=par: e.tensor_tensor(out=xr[:, b, :], in0=xr[:, b, :], in1=tmp3[par][:], op=ALU.add),
                          reads=[tg + "tmp", "xr%d" % b], writes=["xr%d" % b])
                    dma("sp", out[rows, :], xr[:, b, :], ["xr%d" % b], ["out_d"])
                P.flush()

    if "A" in phases:
        phase_A()
    if "B" in phases:
        phase_B()
    if "C" in phases:
        phase_C()
    if "D" in phases:
        phase_D()
    if "E" in phases:
        phase_E()
    scratch = dict(modv=modv, x_tok=x_tok, B_tok=B_tok, BT=BT, CT=CT, sz=sz, dts=dts, hb=hb, x1=x1, hf0=hf0)
    return nc, scratch


_CACHE = {}


def _prep_inputs(inputs, b, T):
    f = lambda a: np.ascontiguousarray(a, dtype=np.float32)
    m = {
        "x": f(inputs["x"][b, :T]),
        "c": f(inputs["c"][b]),
        "ctx": f(inputs["ctx"][b]),
        "c_ctx": f(inputs["c_ctx"]),
        "ada_w": f(inputs["ada_w"]),
        "ada_b": f(inputs["ada_b"]),
        "norm_pre": f(np.reshape(inputs["norm_pre"], (2, 2 * D))),
        "norm_post": f(np.reshape(inputs["norm_post"], (2, 2 * D))),
        "ssd_w_in": f(inputs["ssd_w_in"][0]),
        "ssd_conv_w": f(inputs["ssd_conv_w"][0]),
        "ssd_conv_b": f(inputs["ssd_conv_b"][0]),
        "ssd_a_log": f(np.reshape(inputs["ssd_a_log"][0], (1, 64))),
        "ssd_dt_bias": f(np.reshape(inputs["ssd_dt_bias"][0], (1, 64))),
        "ssd_d": f(np.reshape(inputs["ssd_d"][0], (1, 32))),
        "ssd_norm": f(np.reshape(inputs["ssd_norm"][0], (1, DI))),
        "ssd_w_out": f(inputs["ssd_w_out"][0]),
        "sc_w_in": f(inputs["sc_w_in"][0]),
        "sc_conv_w": f(inputs["sc_conv_w"][0]),
        "sc_w_out": f(inputs["sc_w_out"][0]),
        "ffn_w_gu": f(inputs["ffn_w_gu"][0]),
        "ffn_w_down": f(inputs["ffn_w_down"][0]),
        "moe_router": f(inputs["moe_router"][0]),
        "moe_w_gu": f(inputs["moe_w_gu"][0]),
        "moe_w_down": f(inputs["moe_w_down"][0]),
    }
    return m


def kernel(**inputs):
    x = np.asarray(inputs["x"])
    Bn, T, _ = x.shape
    if T not in _CACHE:
        _CACHE[T] = build(T)[0]
    nc = _CACHE[T]
    shared = _prep_inputs(inputs, 0, T)
    in_maps = []
    for b in range(Bn):
        m = dict(shared)
        m["x"] = np.ascontiguousarray(x[b], dtype=np.float32)
        m["c"] = np.ascontiguousarray(np.asarray(inputs["c"])[b], dtype=np.float32)
        m["ctx"] = np.ascontiguousarray(np.asarray(inputs["ctx"])[b], dtype=np.float32)
        in_maps.append(m)
    res = run_bass_kernel_spmd(nc, in_maps, core_ids=list(range(Bn)))
    return np.stack([np.asarray(r["out"], dtype=np.float32) for r in res.results], axis=0)
```
